# Optimizing a Trainium2 kernel written in Bass

```python
import math
import jax, jax.numpy as jnp
from jax import lax
import numpy as np

D_MODEL = 1024
BATCH = 16
SEQ = 2048
DEPTH = 2

CHUNK = 64
Q_BLOCK = 128
N_MIXERS = 4
GROUP_WIDTH = D_MODEL // N_MIXERS
HEAD_DIM = 64
HEADS_PER_GROUP = GROUP_WIDTH // HEAD_DIM
CONV_WIDTH = 3
SGU_BLOCK = 128
PROJ_WIDTH = 11 * GROUP_WIDTH + HEADS_PER_GROUP
N_EXPERT_GROUPS = 4
EXPERTS_PER_GROUP = 8
N_EXPERTS = N_EXPERT_GROUPS * EXPERTS_PER_GROUP
TOP_K = 2
D_EXPERT = D_MODEL // 4
ROW_BLOCK = 256
ALPHA = (2.0 * DEPTH) ** 0.25
BETA = (8.0 * DEPTH) ** -0.25
LN_EPS = 1e-5
RMS_EPS = 1e-6

kernel_name = "chunk_causal_hybrid_headgroup_moe_encoder"


def layer_norm(x, g, b):
    xf = x.astype(jnp.float32)
    mu = jnp.mean(xf, axis=-1, keepdims=True)
    var = jnp.mean(jnp.square(xf - mu), axis=-1, keepdims=True)
    y = (xf - mu) * lax.rsqrt(var + LN_EPS) * g.astype(jnp.float32) + b.astype(jnp.float32)
    return y.astype(x.dtype)


def group_rms_norm(x, g):
    B, S, D = x.shape
    xf = x.astype(jnp.float32).reshape(B, S, N_MIXERS, GROUP_WIDTH)
    xf = xf * lax.rsqrt(jnp.mean(jnp.square(xf), axis=-1, keepdims=True) + RMS_EPS)
    return (xf.reshape(B, S, D) * g.astype(jnp.float32)).astype(x.dtype)


def to_heads(t):
    B, S, _ = t.shape
    return t.reshape(B, S, HEADS_PER_GROUP, HEAD_DIM).transpose(0, 2, 1, 3)


def from_heads(t):
    B, H, S, d = t.shape
    return t.transpose(0, 2, 1, 3).reshape(B, S, H * d)


def stick_breaking_attention(q, k, v):
    B, H, S, d = q.shape
    nq = S // Q_BLOCK
    qb = q.reshape(B, H, nq, Q_BLOCK, d).transpose(2, 0, 1, 3, 4)
    k_pos = jnp.arange(S)
    scale = 1.0 / math.sqrt(d)

    def block(args):
        q_blk, i = args
        z = jnp.einsum('bhqd,bhkd->bhqk', q_blk, k).astype(jnp.float32) * scale
        q_pos = i * Q_BLOCK + jnp.arange(Q_BLOCK)
        mask = k_pos[None, :] < q_pos[:, None]
        log_1m = jnp.where(mask, jax.nn.log_sigmoid(-z), 0.0)
        later = lax.cumsum(log_1m, axis=3, reverse=True) - log_1m
        w = jnp.where(mask, jnp.exp(jax.nn.log_sigmoid(z) + later), 0.0)
        return jnp.einsum('bhqk,bhkd->bhqd', w.astype(v.dtype), v)

    out = lax.map(block, (qb, jnp.arange(nq)))
    return out.transpose(1, 2, 0, 3, 4).reshape(B, H, S, d)


def forgetting_attention(q, k, v, log_f):
    B, H, S, d = q.shape
    nq = S // Q_BLOCK
    c = lax.cumsum(log_f, axis=2)
    qb = q.reshape(B, H, nq, Q_BLOCK, d).transpose(2, 0, 1, 3, 4)
    cb = c.reshape(B, H, nq, Q_BLOCK).transpose(2, 0, 1, 3)
    k_pos = jnp.arange(S)
    scale = 1.0 / math.sqrt(d)
    neg = jnp.finfo(jnp.float32).min

    def block(args):
        q_blk, c_blk, i = args
        s = jnp.einsum('bhqd,bhkd->bhqk', q_blk, k).astype(jnp.float32) * scale
        s = s + c_blk[..., :, None] - c[:, :, None, :]
        q_pos = i * Q_BLOCK + jnp.arange(Q_BLOCK)
        mask = k_pos[None, :] <= q_pos[:, None]
        p = jax.nn.softmax(jnp.where(mask, s, neg), axis=-1)
        return jnp.einsum('bhqk,bhkd->bhqd', p.astype(v.dtype), v)

    out = lax.map(block, (qb, cb, jnp.arange(nq)))
    return out.transpose(1, 2, 0, 3, 4).reshape(B, H, S, d)


def short_gated_conv(h, b_gate, c_gate, conv_w, conv_b):
    S = h.shape[1]
    z = c_gate * h
    zp = jnp.pad(z, ((0, 0), (CONV_WIDTH - 1, 0), (0, 0)))
    y = conv_b + conv_w[0] * zp[:, 0:S]
    for tap in range(1, CONV_WIDTH):
        y = y + conv_w[tap] * zp[:, tap:tap + S]
    return b_gate * y


def spatial_gating(u, v, ln_g, ln_b, w_s, b_s):
    v = layer_norm(v, ln_g, ln_b)
    B, S, W = v.shape
    nb = S // SGU_BLOCK
    vb = v.reshape(B, nb, SGU_BLOCK, HEADS_PER_GROUP, HEAD_DIM)
    chunk_id = jnp.arange(SGU_BLOCK) // CHUNK
    mask = chunk_id[None, :] <= chunk_id[:, None]
    w_m = jnp.where(mask[None], w_s, 0.0).astype(v.dtype)
    mixed = jnp.einsum('gij,bnjgc->bnigc', w_m, vb) + b_s.T[None, None, :, :, None]
    return u * mixed.reshape(B, S, W)


def mixer_sublayer(x, w_in, b_f, conv_w, conv_b, sgu_ln_g, sgu_ln_b, sgu_w, sgu_b, grp_g, w_out):
    W = GROUP_WIDTH
    proj = x @ w_in
    qkv_a, qkv_b, f_b, conv_in, sgu_in = jnp.split(
        proj, [3 * W, 6 * W, 6 * W + HEADS_PER_GROUP, 9 * W + HEADS_PER_GROUP], axis=-1)
    qa, ka, va = jnp.split(qkv_a, 3, axis=-1)
    out_a = from_heads(stick_breaking_attention(to_heads(qa), to_heads(ka), to_heads(va)))
    qb, kb, vb = jnp.split(qkv_b, 3, axis=-1)
    log_f = jax.nn.log_sigmoid(f_b.astype(jnp.float32) + b_f.astype(jnp.float32)).transpose(0, 2, 1)
    out_b = from_heads(forgetting_attention(to_heads(qb), to_heads(kb), to_heads(vb), log_f))
    h_c, b_c, c_c = jnp.split(conv_in, 3, axis=-1)
    out_c = short_gated_conv(h_c, b_c, c_c, conv_w, conv_b)
    u_d, v_d = jnp.split(jax.nn.gelu(sgu_in), 2, axis=-1)
    out_d = spatial_gating(u_d, v_d, sgu_ln_g, sgu_ln_b, sgu_w, sgu_b)
    mix = group_rms_norm(jnp.concatenate([out_a, out_b, out_c, out_d], axis=-1), grp_g)
    return mix @ w_out


def hierarchical_moe(x, router_g_w, router_g_b, router_e_w, router_e_b, w1, w3, w2):
    Bsz, S, D = x.shape
    xf = x.reshape(-1, D)
    T = xf.shape[0]
    p_g = jax.nn.softmax((xf @ router_g_w).astype(jnp.float32) + router_g_b.astype(jnp.float32), axis=-1)
    g_sel = jnp.argmax(p_g, axis=-1)
    p_gsel = jnp.take_along_axis(p_g, g_sel[:, None], axis=-1)
    e_logits = ((xf @ router_e_w).astype(jnp.float32) + router_e_b.astype(jnp.float32)).reshape(
        T, N_EXPERT_GROUPS, EXPERTS_PER_GROUP)
    e_in = jnp.take_along_axis(e_logits, g_sel[:, None, None], axis=1)[:, 0]
    top_p, top_i = lax.top_k(jax.nn.softmax(e_in, axis=-1), TOP_K)
    gate = p_gsel * top_p / jnp.sum(top_p, axis=-1, keepdims=True)
    expert = g_sel[:, None] * EXPERTS_PER_GROUP + top_i

    A = T * TOP_K
    e_flat = expert.reshape(-1)
    tok_flat = jnp.repeat(jnp.arange(T, dtype=jnp.int32), TOP_K)
    w_flat = gate.reshape(-1)
    order = jnp.argsort(e_flat)
    e_s, tok_s, w_s = e_flat[order], tok_flat[order], w_flat[order]
    counts = jax.ops.segment_sum(jnp.ones((A,), jnp.int32), e_flat, num_segments=N_EXPERTS)
    starts = jnp.cumsum(counts) - counts
    padded = (counts + ROW_BLOCK - 1) // ROW_BLOCK * ROW_BLOCK
    p_ends = jnp.cumsum(padded)
    p_starts = p_ends - padded
    dest = p_starts[e_s] + jnp.arange(A, dtype=jnp.int32) - starts[e_s]
    P = -(-A // ROW_BLOCK) * ROW_BLOCK + N_EXPERTS * ROW_BLOCK
    n_blocks = P // ROW_BLOCK
    tok_pad = jnp.zeros((P,), jnp.int32).at[dest].set(tok_s)
    w_pad = jnp.zeros((P,), jnp.float32).at[dest].set(w_s)
    block_expert = jnp.clip(jnp.searchsorted(p_ends, jnp.arange(n_blocks) * ROW_BLOCK, side='right'),
                            0, N_EXPERTS - 1)
    xs = xf[tok_pad].reshape(n_blocks, ROW_BLOCK, D)

    def expert_block(args):
        xb, e = args
        h = jax.nn.silu(xb @ w1[e]) * (xb @ w3[e])
        return h @ w2[e]

    ys = lax.map(expert_block, (xs, block_expert)).reshape(P, D)
    y = jax.ops.segment_sum(ys * w_pad[:, None].astype(ys.dtype), tok_pad, num_segments=T)
    return y.reshape(Bsz, S, D)


def setup_inputs(seed: int = 0) -> dict:
    key = jax.random.key(seed)
    ks = jax.random.split(key, 24)
    f32 = jnp.float32
    n = lambda k, shape: jax.random.normal(k, shape, f32)
    D, W, G, L = D_MODEL, GROUP_WIDTH, HEADS_PER_GROUP, DEPTH
    return {
        "x": n(ks[0], (BATCH, SEQ, D)),
        "ln_in_g": 1.0 + 0.02 * n(ks[1], (D,)),
        "ln_in_b": 0.02 * n(ks[2], (D,)),
        "w_in": n(ks[3], (L, D, PROJ_WIDTH)) * D ** -0.5,
        "b_f": 2.0 + 0.5 * n(ks[4], (L, G)),
        "conv_w": 0.5 * n(ks[5], (L, CONV_WIDTH, W)),
        "conv_b": 0.02 * n(ks[6], (L, W)),
        "sgu_ln_g": 1.0 + 0.02 * n(ks[7], (L, W)),
        "sgu_ln_b": 0.02 * n(ks[8], (L, W)),
        "sgu_w": n(ks[9], (L, G, SGU_BLOCK, SGU_BLOCK)) * SGU_BLOCK ** -0.5,
        "sgu_b": 1.0 + 0.02 * n(ks[10], (L, G, SGU_BLOCK)),
        "grp_g": 1.0 + 0.02 * n(ks[11], (L, D)),
        "w_out": n(ks[12], (L, D, D)) * D ** -0.5 * BETA,
        "ln1_g": 1.0 + 0.02 * n(ks[13], (L, D)),
        "ln1_b": 0.02 * n(ks[14], (L, D)),
        "router_g_w": n(ks[15], (L, D, N_EXPERT_GROUPS)) * D ** -0.5,
        "router_g_b": 0.01 * n(ks[16], (L, N_EXPERT_GROUPS)),
        "router_e_w": n(ks[17], (L, D, N_EXPERTS)) * D ** -0.5,
        "router_e_b": 0.01 * n(ks[18], (L, N_EXPERTS)),
        "w1": n(ks[19], (L, N_EXPERTS, D, D_EXPERT)) * D ** -0.5,
        "w3": n(ks[20], (L, N_EXPERTS, D, D_EXPERT)) * D ** -0.5,
        "w2": n(ks[21], (L, N_EXPERTS, D_EXPERT, D)) * D_EXPERT ** -0.5 * BETA,
        "ln2_g": 1.0 + 0.02 * n(ks[22], (L, D)),
        "ln2_b": 0.02 * n(ks[23], (L, D)),
    }


def reference(x, ln_in_g, ln_in_b, w_in, b_f, conv_w, conv_b, sgu_ln_g, sgu_ln_b, sgu_w, sgu_b,
              grp_g, w_out, ln1_g, ln1_b, router_g_w, router_g_b, router_e_w, router_e_b,
              w1, w3, w2, ln2_g, ln2_b):
    x = layer_norm(x, ln_in_g, ln_in_b)
    for i in range(DEPTH):
        mixed = mixer_sublayer(x, w_in[i], b_f[i], conv_w[i], conv_b[i], sgu_ln_g[i], sgu_ln_b[i],
                               sgu_w[i], sgu_b[i], grp_g[i], w_out[i])
        x = layer_norm(ALPHA * x + mixed, ln1_g[i], ln1_b[i])
        ffn = hierarchical_moe(x, router_g_w[i], router_g_b[i], router_e_w[i], router_e_b[i],
                               w1[i], w3[i], w2[i])
        x = layer_norm(ALPHA * x + ffn, ln2_g[i], ln2_b[i])
    return x
```

```python
import math
from contextlib import ExitStack
import numpy as np
import ml_dtypes
import concourse.bass as bass
import concourse.mybir as mybir
from concourse.bass_utils import run_bass_kernel_spmd

F32 = mybir.dt.float32
BF16 = mybir.dt.bfloat16
I32 = mybir.dt.int32
AF = mybir.ActivationFunctionType
ALU = mybir.AluOpType
AX = mybir.AxisListType

NCORES = 8
L = 2
D = 1024
S = 2048
NSEQ = 2
T = NSEQ * S
NT = T // 128
PW = 2820
NE = 32
RB = 512
NSLOT = 47
NRT = RB // 128
PROWS = NSLOT * RB
ALPHA = (2.0 * L) ** 0.25
LN_EPS = 1e-5
RMS_EPS = 1e-6
DEBUG_ALLOC = False
PE_WARM_A = 1
PE_WARM_B = 0


class KB:
    ENG = ('pe', 'act', 'dve', 'pool', 'sp')
    EPOCH = 30000

    def __init__(self, nc):
        self.nc = nc
        self.e = {'pe': nc.tensor, 'act': nc.scalar, 'dve': nc.vector, 'pool': nc.gpsimd, 'sp': nc.sync}
        self.semh = {}
        self.cur = {}
        self.cnt = {}
        self.ep = {k: -1 for k in self.ENG}
        for k in self.ENG:
            self._new_epoch(k)
        self.seen = {k: {} for k in self.ENG}
        self.last_w = {}
        self.readers = {}
        self.tokclock = {}
        self.nwaits = 0
        self.ninstr = {k: 0 for k in self.ENG}
        self.lazy = set()

    def _new_epoch(self, k):
        self.ep[k] += 1
        name = '%s#%d' % (k, self.ep[k])
        self.semh[name] = self.nc.alloc_semaphore('s_%s_%d' % (k, self.ep[k]))
        self.cur[k] = name
        self.cnt[name] = 0

    def _group(self, g):
        name = 'd:' + g
        if name not in self.semh:
            self.semh[name] = self.nc.alloc_semaphore('sd_' + g)
            self.cnt[name] = 0
        return name

    def _deps(self, reads, writes):
        deps = {}

        def add(s, v):
            if deps.get(s, 0) < v:
                deps[s] = v
        for k in reads:
            t = self.last_w.get(k)
            if t is not None:
                add(*t)
        for k in writes:
            t = self.last_w.get(k)
            if t is not None:
                add(*t)
            for s, v in self.readers.get(k, {}).items():
                add(s, v)
        return deps

    def _wait(self, engine, deps):
        eng = self.e[engine]
        seen = self.seen[engine]
        for s, v in deps.items():
            if engine == 'pe' and s.startswith('pe#'):
                continue
            if seen.get(s, 0) >= v:
                continue
            eng.wait_ge(self.semh[s], v)
            self.nwaits += 1
            for s2, v2 in self.tokclock.get((s, v), {}).items():
                if seen.get(s2, 0) < v2:
                    seen[s2] = v2
            seen[s] = v

    def _record(self, tok, reads, writes):
        for k in writes:
            self.last_w[k] = tok
            self.readers[k] = {}
        for k in reads:
            r = self.readers.setdefault(k, {})
            if r.get(tok[0], 0) < tok[1]:
                r[tok[0]] = tok[1]

    def op(self, engine, fn, reads=(), writes=()):
        self._wait(engine, self._deps(reads, writes))
        ins = fn(self.e[engine])
        name = self.cur[engine]
        self.cnt[name] += 1
        ins.then_inc(self.semh[name], 1)
        tok = (name, self.cnt[name])
        ck = dict(self.seen[engine])
        ck[name] = self.cnt[name]
        self.tokclock[tok] = ck
        self._record(tok, reads, writes)
        self.ninstr[engine] += 1
        if self.cnt[name] >= self.EPOCH:
            self._new_epoch(engine)
        return tok

    def dma(self, queue, group, fn, reads=(), writes=()):
        self._wait(queue, self._deps(reads, writes))
        ins = fn(self.e[queue])
        name = self._group(group)
        self.cnt[name] += 16
        ins.then_inc(self.semh[name], 16)
        tok = (name, self.cnt[name])
        self.tokclock[tok] = dict(self.seen[queue])
        self._record(tok, reads, writes)
        self.ninstr[queue] += 1
        return tok

    def barrier(self, full=False):
        for k in self.ENG:
            eng = self.e[k]
            for name, h in self.semh.items():
                if name in self.lazy and not full:
                    continue
                v = self.cnt[name]
                if v > 0 and self.seen[k].get(name, 0) < v:
                    eng.wait_ge(h, v)
                    self.seen[k][name] = v
        self.last_w = {}
        self.readers = {}
        self.tokclock = {}

    def finish(self):
        sp = self.e['sp']
        for name, h in self.semh.items():
            if self.cnt[name] > 0:
                sp.wait_ge(h, self.cnt[name])


def _consts():
    bf = ml_dtypes.bfloat16
    j = np.arange(128)[:, None]
    t = np.arange(128)[None, :]
    c = {}
    c['c_ident'] = np.eye(128, dtype=np.float32).astype(bf)
    c['c_identf'] = np.eye(128, dtype=np.float32)
    c['c_mstrict'] = (j < t).astype(np.float32).astype(bf)
    c['c_mincl'] = (j <= t).astype(np.float32).astype(bf)
    c['c_n8tri'] = (-8.0 * (j >= t)).astype(np.float32).astype(bf)
    c['c_negA'] = (-30000.0 * (j >= t)).astype(np.float32).astype(bf)
    c['c_negB'] = (-30000.0 * (j > t)).astype(np.float32).astype(bf)
    c['c_trirank'] = (j < t).astype(np.float32).astype(bf)
    cid = np.arange(128) // 64
    c['c_sgumask'] = (cid[None, :] <= cid[:, None]).astype(np.float32)
    sel = np.zeros((128, 8, 6), np.float32)
    for h in range(4):
        sel[h, h, 0] = 1; sel[32 + h, h, 1] = 1; sel[64 + h, h, 2] = 1
        sel[96, h, 3] = 1; sel[96, h, 4] = 1; sel[96, h, 5] = 1
        sel[96, 4 + h, 0] = 1; sel[96, 4 + h, 1] = 1; sel[96, 4 + h, 2] = 1
        sel[h, 4 + h, 3] = -1; sel[32 + h, 4 + h, 4] = -1; sel[64 + h, 4 + h, 5] = -1
    c['c_sel'] = sel.astype(bf)
    selh = np.zeros((2, 128), np.float32); selh[0, :64] = 1; selh[1, 64:] = 1
    c['c_selh'] = selh
    selden = np.zeros((65, 64), np.float32); selden[64, :] = 1
    c['c_selden'] = selden
    c['c_slotv'] = np.broadcast_to((np.arange(NSLOT, dtype=np.float32) * RB)[None, :], (128, NSLOT)).copy()
    c['c_pidx'] = np.arange(128, dtype=np.float32).reshape(128, 1)
    return c


CONST_SPECS = {
    'c_ident': ([128, 128], BF16), 'c_identf': ([128, 128], F32), 'c_mstrict': ([128, 128], BF16),
    'c_mincl': ([128, 128], BF16), 'c_n8tri': ([128, 128], BF16), 'c_negA': ([128, 128], BF16), 'c_negB': ([128, 128], BF16), 'c_trirank': ([128, 128], BF16),
    'c_sgumask': ([128, 128], F32), 'c_sel': ([128, 8, 6], BF16), 'c_selh': ([2, 128], F32),
    'c_selden': ([65, 64], F32), 'c_slotv': ([128, NSLOT], F32), 'c_pidx': ([128, 1], F32),
}

PARAM_SPECS = {
    'x': [T, D],
    'ln_in_g': [128, D], 'ln_in_b': [128, D],
    'w_in': [L, D, PW],
    'nb_f': [L, 4, 1],
    'conv_w': [L, 128, 2, 3], 'conv_b': [L, 128, 2],
    'sgu_ln_g': [L, 128, 256], 'sgu_ln_b': [L, 128, 256],
    'sgu_w': [L, 4, 128, 128], 'sgu_b': [L, 2, 2, 128],
    'grp_g': [L, 128, 8],
    'w_out': [L, D, D],
    'ln1_g': [L, 128, D], 'ln1_b': [L, 128, D],
    'router_w': [L, D, 36], 'router_b': [L, 128, 36],
    'wexp': [L * NE * 128, 6144],
    'ln2_g': [L, 128, D], 'ln2_b': [L, 128, D],
}


def build_program(phases=None, nlayers=L):
    nc = bass.Bass("TRN2", target_bir_lowering=False)
    kb = KB(nc)
    dr = {}
    for name, shape in PARAM_SPECS.items():
        dr[name] = nc.dram_tensor(name, shape, F32, kind="ExternalInput").ap()
    for name, (shape, dt) in CONST_SPECS.items():
        dr[name] = nc.dram_tensor(name, shape, dt, kind="ExternalInput").ap()
    y_d = nc.dram_tensor("y", [T, D], F32, kind="ExternalOutput").ap()
    xa_d = nc.dram_tensor("xa", [T, D], F32, kind="Internal").ap()
    xb_d = nc.dram_tensor("xb", [T, D], F32, kind="Internal").ap()
    xs_d = nc.dram_tensor("xs", [PROWS, D], BF16, kind="Internal").ap()
    ys_d = nc.dram_tensor("ys", [PROWS, D], BF16, kind="Internal").ap()
    lg_d = nc.dram_tensor("lgd", [NT, 128, 36], F32, kind="Internal").ap()
    xt_d = nc.dram_tensor("xtd", [NSEQ, 128, 8, S], BF16, kind="Internal").ap()

    es_global = ExitStack()
    wexp_bound = nc.gpsimd.to_reg(L * NE * 128 - 1)
    uid = [0]

    def mk_sb(es):
        def sb(name, shape, dt):
            uid[0] += 1
            r = es.enter_context(nc.sbuf_tensor("%s_%d" % (name, uid[0]), shape, dt)).ap()
            if DEBUG_ALLOC:
                print('alloc', name, shape, dt, 'remaining', nc.sbuf_bytes_remaining)
            return r
        return sb
    gsb = mk_sb(es_global)
    PS = [es_global.enter_context(nc.psum_tensor("ps%d" % i, [128, 512], F32)).ap() for i in range(8)]

    def pkey(i):
        return ('ps', i)

    ident = gsb("ident", [128, 128], BF16)
    identf = gsb("identf", [128, 128], F32)
    mstrict = gsb("mstrict", [128, 128], BF16)
    mincl = gsb("mincl", [128, 128], BF16)
    n8tri = gsb("n8tri", [128, 128], BF16)
    negA = gsb("negA", [128, 128], BF16)
    negB = gsb("negB", [128, 128], BF16)
    n8ones = gsb("n8ones", [128, 128], BF16)
    trirank = gsb("trirank", [128, 128], BF16)
    onesb = gsb("onesb", [128, 128], BF16)
    ones256 = gsb("ones256", [128, 128], BF16)
    csel = gsb("csel", [128, 8, 6], BF16)
    epsln = gsb("epsln", [128, 1], F32)
    epsrms = gsb("epsrms", [128, 1], F32)
    for nm, ap in (('c_ident', ident), ('c_identf', identf), ('c_mstrict', mstrict), ('c_mincl', mincl),
                   ('c_n8tri', n8tri), ('c_trirank', trirank), ('c_sel', csel), ('c_negA', negA), ('c_negB', negB)):
        kb.dma('sp', 'const', lambda e, ap=ap, nm=nm: e.dma_start(out=ap, in_=dr[nm]), writes=[nm])
    zrow = gsb("zrow", [128, D], BF16)
    kb.op('pool', lambda e: e.memset(zrow, 0.0), writes=['zrow'])
    kb.lazy.add('d:fill')

    def fill_xs_pieces():
        def mk(r0):
            return lambda: kb.dma('sp', 'fill', lambda e: e.dma_start(out=xs_d[r0:r0 + 128, :], in_=zrow), reads=['zrow'], writes=[('xsfill', r0)])
        return [mk(r0) for r0 in range(0, PROWS, 128)]

    kb.op('pool', lambda e: e.memset(n8ones, -8.0), writes=['n8ones'])
    kb.op('pool', lambda e: e.memset(onesb, 1.0), writes=['onesb'])
    kb.op('pool', lambda e: e.memset(ones256, 1.0 / 256.0), writes=['ones256'])
    kb.op('pool', lambda e: e.memset(epsln, LN_EPS), writes=['epsln'])
    kb.op('pool', lambda e: e.memset(epsrms, RMS_EPS), writes=['epsrms'])
    CK = ['c_ident', 'c_identf', 'c_mstrict', 'c_mincl', 'c_n8tri', 'c_trirank', 'c_sel',
          'n8ones', 'onesb', 'ones256', 'epsln', 'epsrms']

    rr = {'ev': 0}

    def evac_engine():
        rr['ev'] += 1
        return 'act' if rr['ev'] % 2 else 'dve'

    def copy_op(engine, out, in_, reads, writes):
        if engine == 'act':
            kb.op('act', lambda e: e.activation(out=out, in_=in_, func=AF.Copy), reads, writes)
        else:
            kb.op(engine, lambda e: e.tensor_copy(out=out, in_=in_), reads, writes)

    def mm(out, lhsT, rhs, start, stop, reads, writes):
        kb.op('pe', lambda e: e.matmul(out, lhsT=lhsT, rhs=rhs, start=start, stop=stop, skip_group_check=True),
              reads, writes)

    def ln_stats(sbs, R, Rk, tag):
        st, mv, rs, nm = sbs
        kb.op('dve', lambda e: e.bn_stats(out=st[:, 0:6], in_=R[:, 0:512]), [Rk], [tag + 'st'])
        kb.op('dve', lambda e: e.bn_stats(out=st[:, 6:12], in_=R[:, 512:1024]), [Rk], [tag + 'st'])
        kb.op('dve', lambda e: e.bn_aggr(out=mv, in_=st), [tag + 'st'], [tag + 'mv'])
        kb.op('act', lambda e: e.activation(out=rs, in_=mv[:, 1:2], func=AF.Sqrt, bias=epsln, scale=1.0),
              [tag + 'mv', 'epsln'], [tag + 'rs'])
        kb.op('dve', lambda e: e.reciprocal(out=rs, in_=rs), [tag + 'rs'], [tag + 'rs'])
        kb.op('dve', lambda e: e.scalar_tensor_tensor(out=nm, in0=mv[:, 0:1], scalar=-1.0, in1=rs,
                                                        op0=ALU.mult, op1=ALU.mult), [tag + 'mv', tag + 'rs'], [tag + 'nm'])

    def ln_apply(sbs, R, Rk, Y, Yk, g_bc, b_bc, gk, tag, gain_engine='pool'):
        st, mv, rs, nm = sbs
        kb.op('act', lambda e: e.activation(out=Y, in_=R, func=AF.Identity, scale=rs, bias=nm),
              [Rk, tag + 'rs', tag + 'nm'], [Yk])
        kb.op(gain_engine, lambda e: e.tensor_tensor(out=Y, in0=Y, in1=g_bc, op=ALU.mult), [Yk, gk], [Yk])
        kb.op('dve', lambda e: e.tensor_tensor(out=Y, in0=Y, in1=b_bc, op=ALU.add), [Yk, gk], [Yk])

    def emit_xt_tile(Y, Yk, gt, XBc, XTs, i2):
        sq, tl = gt // 16, gt % 16
        tpx = PS[2].bitcast(BF16)
        kb.op('act', lambda e: e.activation(out=XBc[i2], in_=Y, func=AF.Copy), [Yk], [('XBc', i2)])
        for kc in range(8):
            kb.op('pe', lambda e, kc=kc: e.transpose(out=tpx[:, kc * 128:(kc + 1) * 128], in_=XBc[i2][:, kc * 128:(kc + 1) * 128], identity=ident),
                  [('XBc', i2), 'c_ident'], [pkey(2)])
        kb.op('act', lambda e: e.activation(out=XTs[i2], in_=tpx.rearrange("p (k c) -> p k c", c=128), func=AF.Copy), [pkey(2)], [('XTs', i2)])
        kb.dma('sp', 'xto%d' % i2, lambda e: e.dma_start(out=xt_d[sq][:, :, tl * 128:(tl + 1) * 128], in_=XTs[i2]),
               reads=[('XTs', i2)], writes=[('xtd', gt)])

    def emit_ln(sbs, R, Rk, Y, Yk, g_bc, b_bc, gk, tag, gain_engine='pool'):
        ln_stats(sbs, R, Rk, tag)
        ln_apply(sbs, R, Rk, Y, Yk, g_bc, b_bc, gk, tag, gain_engine)

    def phase_ln0(dst_d):
        with ExitStack() as es:
            sb = mk_sb(es)
            g_bc = sb("l0g", [128, D], F32); b_bc = sb("l0b", [128, D], F32)
            kb.dma('sp', 'l0w', lambda e: e.dma_start(out=g_bc, in_=dr['ln_in_g']), writes=['l0gb'])
            kb.dma('sp', 'l0w', lambda e: e.dma_start(out=b_bc, in_=dr['ln_in_b']), writes=['l0gb'])
            kb.barrier()
            X = [sb("l0x%d" % i, [128, D], F32) for i in range(4)]
            sbs = [(sb("l0st%d" % i, [128, 12], F32), sb("l0mv%d" % i, [128, 2], F32),
                    sb("l0rs%d" % i, [128, 1], F32), sb("l0nm%d" % i, [128, 1], F32)) for i in range(4)]

            XBc0 = [sb("l0xb%d" % i, [128, D], BF16) for i in range(2)]
            XTs0 = [sb("l0xs%d" % i, [128, 8, 128], BF16) for i in range(2)]

            def l0_load(t):
                i = t % 4
                kb.dma('sp', 'l0in%d' % i, lambda e: e.dma_start(out=X[i], in_=dr['x'][t * 128:(t + 1) * 128, :]), writes=[('l0x', i)])

            def l0_fin(t):
                i = t % 4
                ln_apply(sbs[i], X[i], ('l0x', i), X[i], ('l0x', i), g_bc, b_bc, 'l0gb', 'l0_%d' % i, gain_engine='pool' if t % 2 else 'dve')
                kb.dma('sp', 'l0out%d' % i, lambda e: e.dma_start(out=dst_d[t * 128:(t + 1) * 128, :], in_=X[i]),
                       reads=[('l0x', i)], writes=[('xa', t)])
            l0_load(0)
            l0_load(1)
            for t in range(NT):
                i = t % 4
                ln_stats(sbs[i], X[i], ('l0x', i), 'l0_%d' % i)
                if t >= 1:
                    l0_fin(t - 1)
                if t >= 2:
                    emit_xt_tile(X[(t - 2) % 4], ('l0x', (t - 2) % 4), t - 2, XBc0, XTs0, t % 2)
                if t + 2 < NT:
                    l0_load(t + 2)
            l0_fin(NT - 1)
            emit_xt_tile(X[(NT - 2) % 4], ('l0x', (NT - 2) % 4), NT - 2, XBc0, XTs0, 0)
            emit_xt_tile(X[(NT - 1) % 4], ('l0x', (NT - 1) % 4), NT - 1, XBc0, XTs0, 1)
        kb.barrier()

    def phase_mix(l, src_d, dst_d):
        fill_pieces = fill_xs_pieces()
        with ExitStack() as es:
            sb = mk_sb(es)
            w_in_v = dr['w_in'][l].rearrange("(kc p) n -> p kc n", p=128)
            w_out_v = dr['w_out'][l].rearrange("(kc p) n -> p kc n", p=128)
            WOUT = sb("WOUT", [128, 8, D], BF16)
            WG = sb("WG", [128, 8, 1540], BF16)
            WST = [sb("WST%d" % i, [128, 8, 64], F32) for i in range(2)]
            sg_g = sb("sgg", [128, 256], F32); sg_b = sb("sgb", [128, 256], F32)
            cw = sb("cw", [128, 2, 3], F32); cb = sb("cb", [128, 2], F32)
            gg = sb("gg", [128, 8], F32)
            nbf = sb("nbf", [4, 1], F32)
            WRr = sb("WRr", [128, 8, 36], F32)
            RBr = sb("RBr", [128, 36], F32)
            LGT = [sb("LGT%d" % i, [128, 36], F32) for i in range(2)]
            WmT = sb("WmT", [128, 4, 128], BF16)
            BS4 = sb("BS4", [128, 2, 4, 128], F32)
            kb.dma('sp', 'mixp', lambda e: e.dma_start(out=WRr, in_=dr['router_w'][l].rearrange("(kc p) n -> p kc n", p=128)), writes=['mixp'])
            kb.dma('sp', 'mixp', lambda e: e.dma_start(out=RBr, in_=dr['router_b'][l]), writes=['mixp'])
            for ap, nm in ((sg_g, 'sgu_ln_g'), (sg_b, 'sgu_ln_b'), (cw, 'conv_w'),
                           (cb, 'conv_b'), (gg, 'grp_g'), (nbf, 'nb_f')):
                kb.dma('sp', 'mixp', lambda e, ap=ap, nm=nm: e.dma_start(out=ap, in_=dr[nm][l]), writes=['mixp'])
            wst_i = [0]

            def load_cols(dst, dst_key, src_v, c0, c1, d0=0, engines=('pool',), pieces=None):
                dkeys = dst_key if isinstance(dst_key, list) else [dst_key]
                o = 0
                while c0 + o < c1:
                    n = min(64, c1 - c0 - o)
                    i = wst_i[0] % 2
                    wst_i[0] += 1
                    def piece(i=i, o=o, n=n, eng=engines[wst_i[0] % len(engines)]):
                        kb.dma('sp', 'wst%d' % i, lambda e: e.dma_start(out=WST[i][:, :, 0:n], in_=src_v[:, :, c0 + o:c0 + o + n]),
                               writes=[('wst', i)])
                        copy_op(eng, dst[:, :, d0 + o:d0 + o + n], WST[i][:, :, 0:n], [('wst', i)], dkeys)
                    if pieces is None:
                        piece()
                    else:
                        pieces.append(piece)
                    o += n
            load_cols(WOUT, 'WOUT', w_out_v, 0, D, engines=('dve', 'act', 'pool'))
            with ExitStack() as es2:
                sb2 = mk_sb(es2)
                sgm = sb2("sgm", [128, 128], F32)
                ws = sb2("ws", [128, 4, 128], F32)
                wsb = sb2("wsb", [128, 4, 128], BF16)
                bs2 = sb2("bs2", [2, 2, 128], F32)
                selh = sb2("selh", [2, 128], F32)
                kb.dma('sp', 'mixp', lambda e: e.dma_start(out=sgm, in_=dr['c_sgumask']), writes=['sgm'])
                kb.dma('sp', 'mixp', lambda e: e.dma_start(out=selh, in_=dr['c_selh']), writes=['selh'])
                kb.dma('sp', 'mixp', lambda e: e.dma_start(out=ws, in_=dr['sgu_w'][l].rearrange("g i j -> i g j")), writes=['ws'])
                kb.dma('sp', 'mixp', lambda e: e.dma_start(out=bs2, in_=dr['sgu_b'][l].rearrange("c h i -> h c i")), writes=['bs2'])
                kb.barrier()
                kb.op('dve', lambda e: e.tensor_scalar(out=nbf, in0=nbf, scalar1=-1.0, scalar2=None, op0=ALU.mult),
                      ['mixp'], ['mixp'])
                for g in range(4):
                    kb.op('dve', lambda e, g=g: e.tensor_tensor(out=wsb[:, g, :], in0=ws[:, g, :], in1=sgm, op=ALU.mult),
                          ['ws', 'sgm'], ['wsb'])
                tpb = PS[2].bitcast(BF16)
                for g in range(4):
                    kb.op('pe', lambda e, g=g: e.transpose(out=tpb[:, g * 128:(g + 1) * 128], in_=wsb[:, g, :], identity=ident),
                          ['wsb', 'c_ident'], [pkey(2)])
                kb.op('dve', lambda e: e.tensor_copy(out=WmT.rearrange("p g i -> p (g i)"), in_=tpb[:, 0:512]), [pkey(2)], ['WmT'])
                for cc in range(2):
                    mm(PS[3][:, 0:128], selh, bs2[:, cc, :], True, True, ['selh', 'bs2'], [pkey(3)])
                    for r in range(4):
                        kb.op('dve', lambda e, cc=cc, r=r: e.tensor_copy(out=BS4[:, cc, r, :], in_=PS[3][:, 0:128]), [pkey(3)], ['BS4'])
                kb.barrier()

            xT = sb("xT", [128, 8, S], BF16)
            MIX = sb("MIX", [128, 8, S], BF16)
            QK = sb("QK", [128, 8, S], BF16)
            VV = sb("VV", [128, 16, 4, 65], BF16)
            XT32 = [sb("XT32_%d" % i, [128, D], F32) for i in range(2)]
            XTB = [sb("XTB_%d" % i, [128, D], BF16) for i in range(2)]
            SCR = sb("SCR", [128, 5124], F32)
            ET = [SCR[:, u * 512:(u + 1) * 512] for u in range(2)]
            UB = [SCR[:, 1024 + u * 256:1024 + (u + 1) * 256].bitcast(BF16) for u in range(3)]
            PT = [SCR[:, 1792 + u * 256:1792 + (u + 1) * 256].bitcast(BF16) for u in range(2)]
            UCB = [SCR[:, 2304 + u * 256:2304 + (u + 1) * 256].bitcast(BF16) for u in range(6)]
            PTB = [XTB[0][:, 0:512], XTB[0][:, 512:1024], XTB[1][:, 0:512]]
            OSB = [SCR[0:65, 3840:4352], SCR[0:65, 3840:4352]]
            REC = SCR[0:64, 4352:4864]
            HS = SCR[:, 0:512]
            ZB = [SCR[:, 512 + cc * 514:512 + (cc + 1) * 514] for cc in range(2)]
            T1 = SCR[:, 1540:2052]
            UC = SCR[:, 2052:3076].rearrange("p (c n) -> p c n", n=512)
            VG4 = SCR[:, 3076:4100].rearrange("p (t n) -> p t n", n=256)
            T2 = SCR[:, 4100:4612]
            VLN4 = SCR[:, 4612:5124].bitcast(BF16).rearrange("p (t n) -> p t n", n=256)
            selden = sb("selden", [65, 64], F32)
            CS = sb("CS", [128, S], BF16)
            FE = SCR[0:4, 0:512]; FCC = [SCR[0:4, 512:1024], SCR[0:4, 1024:1536]]; F8 = SCR[0:4, 1536:2048]
            QA4 = MIX[:, 4:8, :]

            def VAt(t):
                return XT32[t // 8].bitcast(BF16)[:, (t % 8) * 256:(t % 8 + 1) * 256].rearrange("p (h d) -> p h d", d=64)
            FR = sb("FR", [4, 512], F32); FH = sb("FH", [4, 512], BF16)
            onesF = sb("onesF", [4, 512], F32)
            st4 = sb("vst4", [128, 4, 6], F32); mv4 = sb("vmv4", [128, 4, 2], F32); rs4 = sb("vrs4", [128, 4], F32); nm4 = sb("vnm4", [128, 4], F32)
            MIXN = xT[:, 0:2, :].rearrange("p a (b c) -> p (a b) c", c=512)
            RR = [xT[:, 2 + i, :].bitcast(F32) for i in range(2)]
            SQ = [xT[:, 4, (j % 4) * 512:(j % 4 + 1) * 512] for j in range(8)]
            XTTr = xT[:, 5, :].bitcast(F32).rearrange("p (k c) -> p k c", c=128)
            RSG = [xT[:, 6 + g // 2, (g % 2) * 1024:(g % 2 + 1) * 1024].bitcast(F32) for g in range(4)]
            XR = XT32
            lns = [(sb("ost%d" % i, [128, 12], F32), sb("omv%d" % i, [128, 2], F32), sb("ors%d" % i, [128, 1], F32),
                    sb("onm%d" % i, [128, 1], F32)) for i in range(2)]
            kb.dma('sp', 'selden', lambda e: e.dma_start(out=selden, in_=dr['c_selden']), writes=['selden'])
            kb.op('pool', lambda e: e.memset(onesF, 1.0), writes=['onesF'])
            kb.op('pool', lambda e: e.memset(VV, 1.0), writes=['VV'])
            kb.op('pool', lambda e: e.memset(CS, 0.0), writes=['CS'])
            kb.op('pool', lambda e: e.memset(CS[96:97, :], 1.0), writes=['CS'])
            tpb = PS[2].bitcast(BF16)
            pp = [0]

            def nextbank(banks=(0, 1)):
                pp[0] += 1
                return banks[pp[0] % len(banks)]

            for s in range(NSEQ):
                tok0 = s * S
                for tc in range(4):
                    kb.dma('sp', 'xtl%d' % tc, lambda e, tc=tc: e.dma_start(out=xT[:, :, tc * 512:(tc + 1) * 512], in_=xt_d[s][:, :, tc * 512:(tc + 1) * 512]),
                           writes=[('xT', tc)])

                def proj_fm(col, M, tc, bank, extra=None):
                    for kc in range(8):
                        mm(PS[bank][0:M, 0:512], WG[:, kc, wb[0] + col:wb[0] + col + M], xT[:, kc, tc * 512:(tc + 1) * 512],
                           kc == 0, kc == 7, wgk[0] + [('xT', tc)], [pkey(bank)])

                def proj_tm(col, N, t, bank):
                    for kc in range(8):
                        mm(PS[bank][:, 0:N], xT[:, kc, t * 128:(t + 1) * 128], WG[:, kc, wb[0] + col:wb[0] + col + N],
                           kc == 0, kc == 7, wgk[0] + [('xT', t // 4)], [pkey(bank)])

                wgk = [['WGB']]
                wb = [768]
                if s == 0:
                    load_cols(WG, 'WGB', w_in_v, 768, 1540, d0=768, engines=('dve', 'act', 'pool'))
                    load_cols(WG, 'WGA', w_in_v, 0, 768, engines=('dve', 'act', 'pool'))
                for tc in range(4):
                    b = nextbank()
                    cs = slice(tc * 512, (tc + 1) * 512)
                    FC = FCC[tc % 2]
                    proj_fm(768, 4, tc, b)
                    kb.op('act', lambda e, b=b: e.activation(out=FE, in_=PS[b][0:4, 0:512], func=AF.Exp, bias=nbf, scale=-1.0),
                          [pkey(b), 'mixp'], ['FE'])
                    kb.op('act', lambda e: e.activation(out=FE, in_=FE, func=AF.Ln, bias=1.0, scale=1.0), ['FE'], ['FE'])
                    init = 0.0 if tc == 0 else FCC[(tc - 1) % 2][:, 511:512]
                    kb.op('dve', lambda e, FC=FC, init=init: e.tensor_tensor_scan(out=FC, data0=onesF, data1=FE, initial=init, op0=ALU.mult, op1=ALU.add),
                          ['FE', 'onesF', ('FC', (tc - 1) % 2)], [('FC', tc % 2)])
                    kb.op('dve', lambda e, FC=FC: e.tensor_scalar(out=F8, in0=FC, scalar1=-8.0, scalar2=None, op0=ALU.mult), [('FC', tc % 2)], ['F8'])
                    kb.op('dve', lambda e, cs=cs: e.tensor_copy(out=CS[0:4, cs], in_=F8), ['F8'], ['CS'])
                    kb.op('dve', lambda e, cs=cs: e.tensor_tensor(out=FR, in0=F8, in1=CS[0:4, cs], op=ALU.subtract), ['F8', 'CS'], ['FR'])
                    kb.op('dve', lambda e: e.tensor_copy(out=FH, in_=FR), ['FR'], ['FH'])
                    kb.op('dve', lambda e, cs=cs: e.tensor_copy(out=CS[32:36, cs], in_=FH), ['FH'], ['CS'])
                    kb.op('dve', lambda e: e.tensor_tensor(out=FR, in0=FR, in1=FH, op=ALU.subtract), ['FR', 'FH'], ['FR'])
                    kb.op('dve', lambda e, cs=cs: e.tensor_copy(out=CS[64:68, cs], in_=FR), ['FR'], ['CS'])
                wgk[0] = ['WGA']
                wb[0] = 0
                for tc in range(4):
                    for gi in range(4):
                        b = nextbank()
                        proj_fm(gi * 128, 128, tc, b)
                        copy_op(evac_engine(), QA4[:, gi, tc * 512:(tc + 1) * 512], PS[b][:, 0:512], [pkey(b)], ['QKA'])
                    for tl in range(4):
                        t = tc * 4 + tl
                        b = nextbank()
                        proj_tm(512, 256, t, b)
                        copy_op(evac_engine(), VAt(t), PS[b][:, 0:256].rearrange("p (h d) -> p h d", d=64), [pkey(b)], ['VA'])
                unitsA = []
                grp = 0
                for h in range(4):
                    for c in range(4):
                        k = 0
                        for Sb in range(4 * c + 3, -1, -1):
                            q0 = max(Sb * 128, c * 512)
                            unitsA.append(dict(h=h, c=c, Sb=Sb, q0=q0, N=(c + 1) * 512 - q0, loc=q0 - c * 512, diag=Sb >= 4 * c,
                                               first=(Sb == 4 * c + 3), last=(Sb == 0), k=k, g=grp))
                            k += 1
                        grp += 1
                ZBK = (3, 4, 7)

                def a_s1(j, u):
                    hp, ho = u['h'] // 2, (u['h'] % 2) * 64
                    N, loc = u['N'], u['loc']
                    zb = ZBK[j % 3]
                    zs = PS[zb][:, loc:loc + N]
                    e2, u3 = j % 2, j % 3
                    gb = 3 * (u['g'] % 2)
                    if u['first']:
                        for q in range(3):
                            kb.op('pool', lambda e, q=q: e.memset(UCB[gb + q], 0.0), writes=[('UCB', gb + q)])
                    mm(zs, QA4[ho:ho + 64, 2 + hp, u['Sb'] * 128:(u['Sb'] + 1) * 128], QA4[ho:ho + 64, hp, u['q0']:u['q0'] + N], True, True, ['QKA'], [pkey(zb)])
                    if u['diag']:
                        mm(zs[:, 0:128], ident, negA, False, True, ['c_ident', 'c_negA'], [pkey(zb)])
                    for _ in range(PE_WARM_A):
                        mm(PS[2][64:128, 0:512], n8ones[:, 0:64], xT[:, 0, 0:512], True, True, ['n8ones'], ['ps2hi'])
                    kb.op('act', lambda e: e.activation(out=ET[e2][:, 0:N], in_=zs, func=AF.Exp, scale=0.125), [pkey(zb)], [('ET', e2)])
                    kb.op('act', lambda e: e.activation(out=UB[u3][:, 0:N], in_=ET[e2][:, 0:N], func=AF.Ln, bias=1.0, scale=1.0), [('ET', e2)], [('UB', u3)])
                    if not u['last']:
                        kc_, kn_ = gb + u['k'] % 3, gb + (u['k'] + 1) % 3
                        kb.op('pool', lambda e: e.tensor_tensor(out=UCB[kn_][:, loc:512], in0=UCB[kc_][:, loc:512], in1=UB[u3][:, 0:N], op=ALU.add),
                              [('UCB', kc_), ('UB', u3)], [('UCB', kn_)])

                def a_s2(j, u):
                    N, loc = u['N'], u['loc']
                    zb = ZBK[j % 3]
                    zs = PS[zb][:, loc:loc + N]
                    u3, p2 = j % 3, j % 2
                    mm(zs, n8tri, UB[u3][:, 0:N], False, u['first'], [('UB', u3), 'c_n8tri'], [pkey(zb)])
                    if not u['first']:
                        kc_ = 3 * (u['g'] % 2) + u['k'] % 3
                        mm(zs, n8ones, UCB[kc_][:, loc:loc + N], False, True, [('UCB', kc_), 'n8ones'], [pkey(zb)])
                    kb.op('act', lambda e: e.activation(out=PT[p2][:, 0:N], in_=zs, func=AF.Exp, scale=0.125), [pkey(zb)], [('PT', p2)])

                def a_s3(j, u):
                    hp, ho = u['h'] // 2, (u['h'] % 2) * 64
                    N, loc = u['N'], u['loc']
                    ob = 5
                    mm(PS[ob][0:64, loc:loc + N], VAt(u['Sb'])[:, u['h'], :], PT[j % 2][:, 0:N], u['first'], u['last'], ['VA', ('PT', j % 2)], [pkey(ob)])
                    if u['last']:
                        copy_op('dve', MIX[ho:ho + 64, hp, u['c'] * 512:(u['c'] + 1) * 512], PS[ob][0:64, :], [pkey(ob)], ['MIX'])


                wgk[0] = ['WGB']
                wb[0] = 768
                for tc in range(4):
                    for gi in range(8):
                        b = nextbank()
                        proj_fm(gi * 64, 64, tc, b)
                        mm(PS[b][64:70, 0:512], csel[:, gi, :], CS[:, tc * 512:(tc + 1) * 512], True, True, ['CS', 'c_sel'], [pkey(b)])
                        copy_op(evac_engine(), QK[0:70, gi, tc * 512:(tc + 1) * 512], PS[b][0:70, 0:512], [pkey(b)], ['QK'])
                    for tl in range(4):
                        t = tc * 4 + tl
                        b = nextbank()
                        proj_tm(512, 256, t, b)
                        copy_op(evac_engine(), VV[:, t, :, 0:64], PS[b][:, 0:256].rearrange("p (h d) -> p h d", d=64), [pkey(b)], ['VV'])
                cd_pieces = []
                load_cols(WG, ['WGA', 'WGB'], w_in_v, 1540, 2820, d0=0, pieces=cd_pieces)
                unitsB = []
                grp = 0
                for h in range(4):
                    for c in range(4):
                        for Sb in range(4 * c + 3, -1, -1):
                            q0 = max(Sb * 128, c * 512)
                            unitsB.append(dict(h=h, c=c, Sb=Sb, q0=q0, N=(c + 1) * 512 - q0, loc=q0 - c * 512, diag=Sb >= 4 * c,
                                               first=(Sb == 4 * c + 3), last=(Sb == 0), g=grp))
                        grp += 1

                def b_s1(j, u):
                    h, N, loc = u['h'], u['N'], u['loc']
                    zb = (0, 1)[j % 2]
                    zs = PS[zb][:, loc:loc + N]
                    p3 = j % 3
                    mm(zs, QK[0:70, 4 + h, u['Sb'] * 128:(u['Sb'] + 1) * 128], QK[0:70, h, u['q0']:u['q0'] + N], True, True, ['QK'], [pkey(zb)])
                    if u['diag']:
                        mm(zs[:, 0:128], ident, negB, False, True, ['c_ident', 'c_negB'], [pkey(zb)])
                    for _ in range(PE_WARM_B):
                        mm(PS[2][64:128, 0:512], n8ones[:, 0:64], xT[:, 0, 0:512], True, True, ['n8ones'], ['ps2hi'])
                    kb.op('act', lambda e: e.activation(out=PTB[p3][:, 0:N], in_=zs, func=AF.Exp, scale=0.125), [pkey(zb)], [('PTB', p3)])

                def b_s2(j, u):
                    h, N, loc = u['h'], u['N'], u['loc']
                    hp, ho = h // 2, (h % 2) * 64
                    p3 = j % 3
                    ob = 6
                    mm(PS[ob][0:65, loc:loc + N], VV[:, u['Sb'], h, 0:65], PTB[p3][:, 0:N], u['first'], u['last'], ['VV', ('PTB', p3)], [pkey(ob)])
                    if u['last']:
                        o2 = 0
                        copy_op('act', OSB[o2], PS[ob][0:65, :], [pkey(ob)], [('OS', o2)])
                        mm(PS[2][0:64, :], selden, OSB[o2], True, True, [('OS', o2), 'selden'], [pkey(2)])
                        kb.op('dve', lambda e: e.reciprocal(out=REC, in_=PS[2][0:64, :]), [pkey(2)], ['REC'])
                        kb.op('dve', lambda e: e.tensor_tensor(out=MIX[ho:ho + 64, 2 + hp, u['c'] * 512:(u['c'] + 1) * 512], in0=OSB[o2][0:64, :], in1=REC, op=ALU.mult),
                              [('OS', o2), 'REC'], ['MIX'])

                nA, nB = len(unitsA), len(unitsB)
                for j in range(max(nA, nB) + 2):
                    if j < nA:
                        a_s1(j, unitsA[j])
                    if j < nB:
                        b_s1(j, unitsB[j])
                    if 0 <= j - 1 < nA:
                        a_s2(j - 1, unitsA[j - 1])
                    if 0 <= j - 1 < nB:
                        b_s2(j - 1, unitsB[j - 1])
                    if 0 <= j - 2 < nA:
                        a_s3(j - 2, unitsA[j - 2])
                    if j >= 2 and cd_pieces:
                        cd_pieces.pop(0)()
                    for _ in range(2):
                        if fill_pieces:
                            fill_pieces.pop(0)()
                while cd_pieces:
                    cd_pieces.pop(0)()

                kb.barrier()
                wgk[0] = ['WGA', 'WGB']
                wb[0] = 0
                for cc in range(2):
                    kb.op('pool', lambda e, cc=cc: e.memset(ZB[cc][:, 0:2], 0.0), writes=[('ZB', cc)])
                for tc in range(4):
                    cs = slice(tc * 512, (tc + 1) * 512)
                    for cc in range(2):
                        bh = nextbank((0, 1, 3, 4))
                        proj_fm(cc * 128, 128, tc, bh)
                        copy_op('act', HS, PS[bh][:, 0:512], [pkey(bh)], ['HS'])
                        bc_ = nextbank((0, 1, 3, 4))
                        proj_fm(512 + cc * 128, 128, tc, bc_)
                        kb.op('dve', lambda e, cc=cc, bc_=bc_: e.tensor_tensor(out=ZB[cc][:, 2:514], in0=HS, in1=PS[bc_][:, 0:512], op=ALU.mult),
                              ['HS', pkey(bc_)], [('ZB', cc)])
                        bb = nextbank((0, 1, 3, 4))
                        proj_fm(256 + cc * 128, 128, tc, bb)
                        kb.op('dve', lambda e, cc=cc: e.tensor_scalar(out=T1, in0=ZB[cc][:, 0:512], scalar1=cw[:, cc, 0:1], scalar2=None, op0=ALU.mult),
                              [('ZB', cc), 'mixp'], ['T1'])
                        kb.op('dve', lambda e, cc=cc: e.scalar_tensor_tensor(out=T1, in0=ZB[cc][:, 1:513], scalar=cw[:, cc, 1:2], in1=T1, op0=ALU.mult, op1=ALU.add),
                              [('ZB', cc), 'mixp', 'T1'], ['T1'])
                        kb.op('dve', lambda e, cc=cc: e.scalar_tensor_tensor(out=T1, in0=ZB[cc][:, 2:514], scalar=cw[:, cc, 2:3], in1=T1, op0=ALU.mult, op1=ALU.add),
                              [('ZB', cc), 'mixp', 'T1'], ['T1'])
                        kb.op('dve', lambda e, cc=cc, bb=bb, cs=cs: e.scalar_tensor_tensor(out=MIX[:, 4 + cc, cs], in0=T1, scalar=cb[:, cc:cc + 1], in1=PS[bb][:, 0:512], op0=ALU.add, op1=ALU.mult),
                              ['T1', 'mixp', pkey(bb)], ['MIX'])
                        kb.op('pool', lambda e, cc=cc: e.tensor_copy(out=ZB[cc][:, 0:2], in_=ZB[cc][:, 512:514]), [('ZB', cc)], [('ZB', cc)])
                    for cc in range(2):
                        bu = nextbank((0, 1, 3, 4))
                        proj_fm(768 + cc * 128, 128, tc, bu)
                        kb.op('act', lambda e, cc=cc, bu=bu: e.activation(out=UC[:, cc, :], in_=PS[bu][:, 0:512], func=AF.Gelu_apprx_tanh),
                              [pkey(bu)], ['UC'])
                    for tl in range(4):
                        t = tc * 4 + tl
                        bv = nextbank((0, 1, 3, 4))
                        proj_tm(1024, 256, t, bv)
                        kb.op('act', lambda e, bv=bv, tl=tl: e.activation(out=VG4[:, tl, :], in_=PS[bv][:, 0:256], func=AF.Gelu_apprx_tanh), [pkey(bv)], [('VG', tl)])
                        kb.op('dve', lambda e, tl=tl: e.bn_stats(out=st4[:, tl, :], in_=VG4[:, tl, :]), [('VG', tl)], [('vst', tl)])
                        kb.op('dve', lambda e, tl=tl: e.bn_aggr(out=mv4[:, tl, :], in_=st4[:, tl, :]), [('vst', tl)], [('vmv', tl)])
                    vmk = [('vmv', tl) for tl in range(4)]
                    kb.op('act', lambda e: e.activation(out=rs4, in_=mv4[:, :, 1], func=AF.Sqrt, bias=epsln, scale=1.0), vmk + ['epsln'], ['vrs'])
                    kb.op('dve', lambda e: e.reciprocal(out=rs4, in_=rs4), ['vrs'], ['vrs'])
                    kb.op('dve', lambda e: e.scalar_tensor_tensor(out=nm4, in0=mv4[:, :, 0], scalar=-1.0, in1=rs4, op0=ALU.mult, op1=ALU.mult), vmk + ['vrs'], ['vnm'])
                    for tl in range(4):
                        kb.op('act', lambda e, tl=tl: e.activation(out=VG4[:, tl, :], in_=VG4[:, tl, :], func=AF.Identity, scale=rs4[:, tl:tl + 1], bias=nm4[:, tl:tl + 1]),
                              [('VG', tl), 'vrs', 'vnm'], [('VG', tl)])
                        kb.op('pool', lambda e, tl=tl: e.tensor_tensor(out=VG4[:, tl, :], in0=VG4[:, tl, :], in1=sg_g, op=ALU.mult), [('VG', tl), 'mixp'], [('VG', tl)])
                        kb.op('dve', lambda e, tl=tl: e.tensor_tensor(out=VLN4[:, tl, :], in0=VG4[:, tl, :], in1=sg_b, op=ALU.add), [('VG', tl), 'mixp'], [('VLN', tl)])
                        for g in range(4):
                            bk = 5 + g // 2
                            mm(PS[bk][(g % 2) * 64:(g % 2) * 64 + 64, tl * 128:(tl + 1) * 128], VLN4[:, tl, g * 64:(g + 1) * 64], WmT[:, g, :],
                               True, True, [('VLN', tl), 'WmT'], [pkey(bk)])
                    for cc in range(2):
                        kb.op('dve', lambda e, cc=cc: e.tensor_tensor(out=T2, in0=PS[5 + cc][:, 0:512], in1=BS4[:, cc, :, :].rearrange("p r i -> p (r i)"), op=ALU.add),
                              [pkey(5 + cc), 'BS4'], ['T2'])
                        kb.op('pool', lambda e, cc=cc, cs=cs: e.tensor_tensor(out=MIX[:, 6 + cc, cs], in0=T2, in1=UC[:, cc, :], op=ALU.mult),
                              ['T2', 'UC'], ['MIX'])

                kb.barrier()
                g1 = SCR[:, 0:1024]
                b1 = SCR[:, 1024:2048]
                kb.dma('sp', 'g1b1', lambda e: e.dma_start(out=g1, in_=dr['ln1_g'][l]), writes=['g1b1'])
                kb.dma('sp', 'g1b1', lambda e: e.dma_start(out=b1, in_=dr['ln1_b'][l]), writes=['g1b1'])
                if s + 1 < NSEQ:
                    load_cols(WG, 'WGB', w_in_v, 768, 1540, d0=768)
                    load_cols(WG, 'WGA', w_in_v, 0, 768)
                for tc in range(4):
                    cs = slice(tc * 512, (tc + 1) * 512)
                    GB = (0, 1, 5, 6)
                    for grp in range(4):
                        b = GB[grp]
                        for j in range(2):
                            q = 2 * grp + j
                            kb.op('act', lambda e, q=q, cs=cs: e.activation(out=SQ[q], in_=MIX[:, q, cs], func=AF.Square),
                                  ['MIX'], [('SQ', q % 4)])
                            mm(PS[b][:, 0:512], ones256, SQ[q], j == 0, j == 1, [('SQ', q % 4), 'ones256'], [pkey(b)])
                    for grp in range(4):
                        b = GB[grp]
                        kb.op('act', lambda e, b=b, grp=grp: e.activation(out=RSG[grp], in_=PS[b][:, 0:512], func=AF.Sqrt, bias=epsrms, scale=1.0),
                              [pkey(b), 'epsrms'], [('RS', grp)])
                    for grp in range(4):
                        kb.op('dve', lambda e, grp=grp: e.reciprocal(out=RSG[grp], in_=RSG[grp]), [('RS', grp)], [('RS', grp)])
                        for j in range(2):
                            kc = 2 * grp + j
                            kb.op('dve', lambda e, kc=kc, cs=cs, grp=grp: e.scalar_tensor_tensor(out=MIXN[:, kc, :], in0=MIX[:, kc, cs], scalar=gg[:, kc:kc + 1], in1=RSG[grp], op0=ALU.mult, op1=ALU.mult),
                                  ['MIX', 'mixp', ('RS', grp)], [('MIXN', kc)])
                    for tl in range(4):
                        t = tc * 4 + tl
                        gt = tok0 // 128 + t
                        i = t % 2

                        def xr_load(tt):
                            ii, g_ = tt % 2, tok0 // 128 + tt
                            kb.dma('sp', 'xr%d' % ii, lambda e: e.dma_start(out=XR[ii], in_=src_d[g_ * 128:(g_ + 1) * 128, :]),
                                   reads=[('xa', g_)], writes=[('xt32', ii)])
                        if t == 0:
                            xr_load(0)
                        if t + 1 < 16:
                            xr_load(t + 1)
                        def fin_apply(tt):
                            ii, g_ = tt % 2, tok0 // 128 + tt
                            ln_apply(lns[ii], RR[ii], ('RR', ii), RR[ii], ('RR', ii), g1, b1, 'g1b1', 'ln1_%d' % ii)
                            kb.dma('sp', 'x1o%d' % ii, lambda e: e.dma_start(out=dst_d[g_ * 128:(g_ + 1) * 128, :], in_=RR[ii]),
                                   reads=[('RR', ii)], writes=[('xb', g_)])

                        def fin_router(tt):
                            ii, g_ = tt % 2, tok0 // 128 + tt
                            for hf in range(2):
                                for k4 in range(4):
                                    kc = hf * 4 + k4
                                    kb.op('pe', lambda e, kc=kc, k4=k4: e.transpose(out=PS[2][:, k4 * 128:(k4 + 1) * 128], in_=RR[ii][:, kc * 128:(kc + 1) * 128], identity=identf),
                                          [('RR', ii), 'c_identf'], [pkey(2)])
                                copy_op('act', XTTr[:, hf * 4:(hf + 1) * 4, :], PS[2].rearrange("p (k c) -> p k c", c=128), [pkey(2)], [('XTTr', hf)])
                            for kc in range(8):
                                mm(PS[7][:, 0:36], XTTr[:, kc, :], WRr[:, kc, :], kc == 0, kc == 7, [('XTTr', kc // 4), 'mixp'], [pkey(7)])
                            kb.op('dve', lambda e: e.tensor_tensor(out=LGT[ii], in0=PS[7][:, 0:36], in1=RBr, op=ALU.add), [pkey(7), 'mixp'], [('LGT', ii)])
                            kb.dma('sp', 'lgo%d' % ii, lambda e: e.dma_start(out=lg_d[g_], in_=LGT[ii]), reads=[('LGT', ii)], writes=[('lg', g_)])
                        banks = []
                        for half in range(2):
                            b = nextbank((3, 4))
                            banks.append(b)
                            for kc in range(8):
                                mm(PS[b][:, 0:512], MIXN[:, kc, tl * 128:(tl + 1) * 128], WOUT[:, kc, half * 512:(half + 1) * 512],
                                   kc == 0, kc == 7, [('MIXN', kc), 'WOUT'], [pkey(b)])
                        if t >= 1:
                            fin_apply(t - 1)
                        for half in range(2):
                            b = banks[half]
                            kb.op('dve', lambda e, i=i, half=half, b=b: e.scalar_tensor_tensor(out=RR[i][:, half * 512:(half + 1) * 512], in0=XR[i][:, half * 512:(half + 1) * 512],
                                                                                               scalar=ALPHA, in1=PS[b][:, 0:512], op0=ALU.mult, op1=ALU.add),
                                  [('xt32', i), pkey(b)], [('RR', i)])
                        ln_stats(lns[i], RR[i], ('RR', i), 'ln1_%d' % i)
                        if t >= 1:
                            fin_router(t - 1)
                        if t == 15:
                            fin_apply(15)
                            fin_router(15)
                kb.barrier()
            while fill_pieces:
                fill_pieces.pop(0)()
        kb.barrier()

    def phase_moe(l, src_d, dst_d):
        kb.barrier(full=True)
        with ExitStack() as es:
            sb = mk_sb(es)
            g2 = sb("ln2g", [128, D], F32); b2 = sb("ln2b", [128, D], F32)
            XT = [sb("MX%d" % i, [128, D], F32) for i in range(3)]
            GATE = [sb("gate%d" % k, [128, NT], F32) for k in range(2)]
            DEST = sb("DEST", [128, 2, NT], I32)
            WIDX = sb("WIDX", [128, NSLOT], I32)
            es_r = ExitStack()
            sbr = mk_sb(es_r)
            slotv = sbr("slotv", [128, NSLOT], F32)
            pidx = sbr("pidx", [128, 1], F32)
            for ap, nm in ((g2, 'ln2_g'), (b2, 'ln2_b')):
                kb.dma('sp', 'moep', lambda e, ap=ap, nm=nm: e.dma_start(out=ap, in_=dr[nm][l]), writes=['moep'])
            kb.dma('sp', 'moep', lambda e: e.dma_start(out=slotv, in_=dr['c_slotv']), writes=['moep'])
            kb.dma('sp', 'moep', lambda e: e.dma_start(out=pidx, in_=dr['c_pidx']), writes=['moep'])
            kb.barrier()
            LG = sbr("LG", [128, NT, 36], F32)
            kb.dma('sp', 'lgin', lambda e: e.dma_start(out=LG, in_=lg_d.rearrange("t p n -> p t n")), writes=['LG'])
            def t3(name, a, b_, dt=F32):
                return sb(name, [128, NT, a, b_] if b_ else [128, NT, a], dt)
            lg = LG[:, :, 0:4]
            le = LG[:, :, 4:36].rearrange("p t (g j) -> p t g j", j=8)
            gmax = sbr("gmax", [128, NT], F32)
            G1 = sbr("G1", [128, NT, 4], F32)
            dl = sbr("dl", [128, NT, 4], F32)
            sume = sbr("sume", [128, NT], F32)
            pg = sbr("pg", [128, NT], F32)
            prod = sbr("prod", [128, NT, 4, 8], F32)
            ein = sbr("ein", [128, NT, 8], F32)
            ein2 = sbr("ein2", [128, NT, 8], F32)
            m1 = sbr("m1", [128, NT], F32); m2 = sbr("m2", [128, NT], F32)
            oh = [sbr("oh%d" % k, [128, NT, 8], F32) for k in range(2)]
            OH = [sbr("OH%d" % k, [128, NT, 4, 8], F32) for k in range(2)]
            OHB = [sbr("OHB%d" % k, [128, NT, 32], BF16) for k in range(2)]

            def bc3(ap, n):
                return ap.unsqueeze(2).to_broadcast([128, NT, n])
            V = 'dve'
            kb.op(V, lambda e: e.reduce_max(out=gmax, in_=lg, axis=AX.X), ['LG'], ['gmax'])
            kb.op(V, lambda e: e.tensor_tensor(out=G1, in0=lg, in1=bc3(gmax, 4), op=ALU.is_equal), ['LG', 'gmax'], ['G1'])
            kb.op(V, lambda e: e.tensor_tensor(out=dl, in0=lg, in1=bc3(gmax, 4), op=ALU.subtract), ['LG', 'gmax'], ['dl'])
            kb.op('act', lambda e: e.activation(out=dl, in_=dl, func=AF.Exp), ['dl'], ['dl'])
            kb.op(V, lambda e: e.reduce_sum(out=sume, in_=dl, axis=AX.X), ['dl'], ['sume'])
            kb.op(V, lambda e: e.reciprocal(out=pg, in_=sume), ['sume'], ['pg'])
            kb.op(V, lambda e: e.tensor_tensor(out=prod, in0=le, in1=G1.unsqueeze(3).to_broadcast([128, NT, 4, 8]), op=ALU.mult), ['LG', 'G1'], ['prod'])
            kb.op(V, lambda e: e.reduce_sum(out=ein, in_=prod.rearrange("p t g j -> p t j g"), axis=AX.X), ['prod'], ['ein'])
            kb.op(V, lambda e: e.reduce_max(out=m1, in_=ein, axis=AX.X), ['ein'], ['m1'])
            kb.op(V, lambda e: e.tensor_tensor(out=oh[0], in0=ein, in1=bc3(m1, 8), op=ALU.is_equal), ['ein', 'm1'], ['oh0'])
            kb.op(V, lambda e: e.scalar_tensor_tensor(out=ein2, in0=oh[0], scalar=-1e30, in1=ein, op0=ALU.mult, op1=ALU.add), ['oh0', 'ein'], ['ein2'])
            kb.op(V, lambda e: e.reduce_max(out=m2, in_=ein2, axis=AX.X), ['ein2'], ['m2'])
            kb.op(V, lambda e: e.tensor_tensor(out=oh[1], in0=ein2, in1=bc3(m2, 8), op=ALU.is_equal), ['ein2', 'm2'], ['oh1'])
            kb.op(V, lambda e: e.tensor_tensor(out=m1, in0=m1, in1=m2, op=ALU.subtract), ['m1', 'm2'], ['m1'])
            kb.op('act', lambda e: e.activation(out=m1, in_=m1, func=AF.Sigmoid), ['m1'], ['m1'])
            kb.op(V, lambda e: e.tensor_tensor(out=GATE[0], in0=m1, in1=pg, op=ALU.mult), ['m1', 'pg'], ['gate0'])
            kb.op(V, lambda e: e.tensor_tensor(out=GATE[1], in0=pg, in1=GATE[0], op=ALU.subtract), ['pg', 'gate0'], ['gate1'])
            for k in range(2):
                kb.op(V, lambda e, k=k: e.tensor_tensor(out=OH[k], in0=G1.unsqueeze(3).to_broadcast([128, NT, 4, 8]),
                                                        in1=oh[k].unsqueeze(2).to_broadcast([128, NT, 4, 8]), op=ALU.mult),
                      ['G1', 'oh%d' % k], ['OH%d' % k])
                kb.op(V, lambda e, k=k: e.tensor_copy(out=OHB[k], in_=OH[k].rearrange("p t g j -> p t (g j)")), ['OH%d' % k], ['OHB%d' % k])

            TOT = sbr("TOT", [128, 2, NT, 32], F32)
            CA = sbr("CA", [128, 2, NT, 32], F32)
            CBf = sbr("CBf", [128, 2, NT, 32], F32)
            cnt = sbr("cnt", [128, 32], F32); pad = sbr("pad", [128, 32], F32)
            padi = sbr("padi", [128, 32], I32)
            pe_a = sbr("pe_a", [128, 32], F32); pe_b = sbr("pe_b", [128, 32], F32)
            base = sbr("base", [128, 2, 32], F32)
            OFF = sbr("OFF", [128, NT, 32], F32)
            DESTF = sbr("DESTF", [128, 2, NT], F32)
            for k in range(2):
                for hf in range(2):
                    mm(PS[4 + hf][:, 0:512], onesb, OHB[k].rearrange("p t e -> p (t e)")[:, hf * 512:(hf + 1) * 512], True, True,
                       ['OHB%d' % k, 'onesb'], [pkey(4 + hf)])
                    kb.op(V, lambda e, k=k, hf=hf: e.tensor_copy(out=TOT[:, k, hf * 16:(hf + 1) * 16, :].rearrange("p t e -> p (t e)"), in_=PS[4 + hf][:, 0:512]),
                          [pkey(4 + hf)], ['TOT'])
            kb.op(V, lambda e: e.tensor_copy(out=CA, in_=TOT), ['TOT'], ['CA'])
            src, dst, sk, dk = CA, CBf, 'CA', 'CB'
            sh = 1
            while sh < NT:
                kb.op(V, lambda e, src=src, dst=dst, sh=sh: e.tensor_copy(out=dst[:, :, 0:sh, :], in_=src[:, :, 0:sh, :]), [sk], [dk])
                kb.op(V, lambda e, src=src, dst=dst, sh=sh: e.tensor_tensor(out=dst[:, :, sh:NT, :], in0=src[:, :, sh:NT, :], in1=src[:, :, 0:NT - sh, :], op=ALU.add), [sk], [dk])
                src, dst, sk, dk = dst, src, dk, sk
                sh *= 2
            CUM, cumk = src, sk
            kb.op(V, lambda e: e.tensor_tensor(out=cnt, in0=CUM[:, 0, NT - 1, :], in1=CUM[:, 1, NT - 1, :], op=ALU.add), [cumk], ['cnt'])
            kb.op(V, lambda e: e.tensor_scalar(out=padi, in0=cnt, scalar1=float(RB - 1), scalar2=None, op0=ALU.add), ['cnt'], ['padi'])
            kb.op(V, lambda e: e.tensor_scalar(out=padi, in0=padi, scalar1=9, scalar2=9, op0=ALU.arith_shift_right, op1=ALU.logical_shift_left), ['padi'], ['padi'])
            kb.op(V, lambda e: e.tensor_copy(out=pad, in_=padi), ['padi'], ['pad'])
            kb.op(V, lambda e: e.tensor_copy(out=pe_a, in_=pad), ['pad'], ['pe_a'])
            src, dst, sk, dk = pe_a, pe_b, 'pe_a', 'pe_b'
            sh = 1
            while sh < 32:
                kb.op(V, lambda e, src=src, dst=dst, sh=sh: e.tensor_copy(out=dst[:, 0:sh], in_=src[:, 0:sh]), [sk], [dk])
                kb.op(V, lambda e, src=src, dst=dst, sh=sh: e.tensor_tensor(out=dst[:, sh:32], in0=src[:, sh:32], in1=src[:, 0:32 - sh], op=ALU.add), [sk], [dk])
                src, dst, sk, dk = dst, src, dk, sk
                sh *= 2
            PEND, pendk = src, sk
            kb.op(V, lambda e: e.tensor_tensor(out=base[:, 0, :], in0=PEND, in1=pad, op=ALU.subtract), [pendk, 'pad'], ['base'])
            kb.op(V, lambda e: e.tensor_tensor(out=base[:, 1, :], in0=base[:, 0, :], in1=CUM[:, 0, NT - 1, :], op=ALU.add), ['base', cumk], ['base'])
            for k in range(2):
                kb.op(V, lambda e, k=k: e.tensor_tensor(out=OFF, in0=CUM[:, k, :, :], in1=TOT[:, k, :, :], op=ALU.subtract), [cumk, 'TOT'], ['OFF'])
                kb.op(V, lambda e, k=k: e.tensor_tensor(out=OFF, in0=OFF, in1=base[:, k, :].unsqueeze(1).to_broadcast([128, NT, 32]), op=ALU.add), ['OFF', 'base'], ['OFF'])
                for hf in range(2):
                    mm(PS[4 + hf][:, 0:512], trirank, OHB[k].rearrange("p t e -> p (t e)")[:, hf * 512:(hf + 1) * 512], True, True,
                       ['OHB%d' % k, 'c_trirank'], [pkey(4 + hf)])
                    kb.op(V, lambda e, hf=hf: e.tensor_tensor(out=OFF[:, hf * 16:(hf + 1) * 16, :].rearrange("p t e -> p (t e)"), in0=OFF[:, hf * 16:(hf + 1) * 16, :].rearrange("p t e -> p (t e)"),
                                                              in1=PS[4 + hf][:, 0:512], op=ALU.add), ['OFF', pkey(4 + hf)], ['OFF'])
                kb.op(V, lambda e, k=k: e.tensor_tensor(out=OFF, in0=OFF, in1=OH[k].rearrange("p t g j -> p t (g j)"), op=ALU.mult), ['OFF', 'OH%d' % k], ['OFF'])
                kb.op(V, lambda e, k=k: e.reduce_sum(out=DESTF[:, k, :], in_=OFF, axis=AX.X), ['OFF'], ['DESTF'])
            kb.op(V, lambda e: e.tensor_copy(out=DEST, in_=DESTF), ['DESTF'], ['DEST'])
            CMP = sbr("CMP", [128, NSLOT, 32], F32)
            SE = sbr("SE", [128, NSLOT], F32)
            SEO = sbr("SEO", [128, NSLOT], F32)
            kb.op(V, lambda e: e.tensor_tensor(out=CMP, in0=PEND.unsqueeze(1).to_broadcast([128, NSLOT, 32]), in1=slotv.unsqueeze(2).to_broadcast([128, NSLOT, 32]), op=ALU.is_le),
                  [pendk, 'moep'], ['CMP'])
            kb.op(V, lambda e: e.reduce_sum(out=SE, in_=CMP, axis=AX.X), ['CMP'], ['SE'])
            kb.op(V, lambda e: e.tensor_scalar(out=SEO, in0=SE, scalar1=float(NE) - 0.5, scalar2=1.0e6, op0=ALU.is_ge, op1=ALU.mult), ['SE'], ['SEO'])
            kb.op(V, lambda e: e.tensor_scalar(out=SE, in0=SE, scalar1=float(NE - 1), scalar2=float(l * NE), op0=ALU.min, op1=ALU.add), ['SE'], ['SE'])
            kb.op(V, lambda e: e.scalar_tensor_tensor(out=SE, in0=SE, scalar=128.0, in1=pidx.to_broadcast([128, NSLOT]), op0=ALU.mult, op1=ALU.add), ['SE', 'moep'], ['SE'])
            kb.op(V, lambda e: e.tensor_tensor(out=SE, in0=SE, in1=SEO, op=ALU.add), ['SE', 'SEO'], ['SE'])
            kb.op(V, lambda e: e.tensor_copy(out=WIDX, in_=SE), ['SE'], ['WIDX'])

            kb.barrier()
            es_r.close()
            XB = [sb("XBs%d" % i, [128, D], BF16) for i in range(2)]
            for t in range(NT):
                i = t % 2
                kb.dma('sp', 'mx%d' % i, lambda e, i=i, t=t: e.dma_start(out=XT[i], in_=src_d[t * 128:(t + 1) * 128, :]),
                       reads=[('xb', t)], writes=[('MX', i)])
                copy_op('act' if i else 'dve', XB[i], XT[i], [('MX', i)], [('XBs', i)])
                for k in range(2):
                    kb.dma('pool', 'scat%d' % i, lambda e, i=i, k=k, t=t: e.indirect_dma_start(
                        out=xs_d, out_offset=bass.IndirectOffsetOnAxis(ap=DEST[:, k, t:t + 1], axis=0), in_=XB[i], in_offset=None),
                        reads=[('XBs', i), 'DEST'], writes=[('xs', t, k)])

            kb.barrier()
            WS = [sb("WS%d" % i, [128, 6144], F32) for i in range(2)]
            WB = [sb("WB%d" % i, [128, 6144], BF16) for i in range(2)]
            XS = [sb("XS%d" % i, [128, NRT, D], BF16) for i in range(2)]
            XST = [sb("XST%d" % i, [128, 8, RB], BF16) for i in range(2)]
            SG = sb("SG", [128, 2, RB], F32)
            HT = sb("HT", [128, 2, RB], BF16)
            YS = [sb("YS%d" % i, [128, NRT, D], BF16) for i in range(2)]
            pq = [0]

            def slot_w(sl):
                i = sl % 2
                kb.dma('pool', 'wexp%d' % i, lambda e: e.indirect_dma_start(
                    out=WS[i], out_offset=None, in_=dr['wexp'], in_offset=bass.IndirectOffsetOnAxis(ap=WIDX[:, sl:sl + 1], axis=0),
                    bounds_check=wexp_bound, oob_is_err=False),
                    reads=['WIDX'], writes=[('WS', i)])

            def slot_x(sl):
                i = sl % 2
                kb.dma('sp', 'xsl%d' % i, lambda e: e.dma_start(out=XS[i], in_=xs_d[sl * RB:(sl + 1) * RB, :].rearrange("(r p) d -> p r d", p=128)),
                       reads=[], writes=[('XS', i)])

            def slot_T(sl):
                i = sl % 2
                for r in range(NRT):
                    tb_ = 2 if r % 2 == 0 else 7
                    tpx = PS[tb_].bitcast(BF16)
                    for kc in range(8):
                        kb.op('pe', lambda e, r=r, kc=kc, tpx=tpx: e.transpose(out=tpx[:, kc * 128:(kc + 1) * 128], in_=XS[i][:, r, kc * 128:(kc + 1) * 128], identity=ident),
                              [('XS', i), 'c_ident'], [pkey(tb_)])
                    copy_op('dve' if r % 2 else 'act', XST[i][:, :, r * 128:(r + 1) * 128], tpx.rearrange("p (k c) -> p k c", c=128), [pkey(tb_)], [('XST', i, r)])

            def slot_cast(sl):
                i = sl % 2
                kb.op('dve', lambda e: e.tensor_copy(out=WB[i][:, 0:2048], in_=WS[i][:, 0:2048]), [('WS', i)], [('WB1', i)])
                kb.op('act', lambda e: e.activation(out=WB[i][:, 2048:4096], in_=WS[i][:, 2048:4096], func=AF.Copy), [('WS', i)], [('WB3', i)])
                kb.op('dve', lambda e: e.tensor_copy(out=WB[i][:, 4096:5120], in_=WS[i][:, 4096:5120]), [('WS', i)], [('WB2a', i)])
                kb.op('act', lambda e: e.activation(out=WB[i][:, 5120:6144], in_=WS[i][:, 5120:6144], func=AF.Copy), [('WS', i)], [('WB2b', i)])

            def slot_H(sl):
                i = sl % 2
                W1 = WB[i][:, 0:2048].rearrange("p (k c) -> p k c", c=256)
                W3 = WB[i][:, 2048:4096].rearrange("p (k c) -> p k c", c=256)
                xk = [('XST', i, r_) for r_ in range(NRT)]
                for dc in range(2):
                    b1_, b3_ = (0, 1) if dc == 0 else (5, 6)
                    for kc in range(8):
                        mm(PS[b1_][:, 0:RB], W1[:, kc, dc * 128:(dc + 1) * 128], XST[i][:, kc, :], kc == 0, kc == 7, [('WB1', i)] + xk, [pkey(b1_)])
                    for kc in range(8):
                        mm(PS[b3_][:, 0:RB], W3[:, kc, dc * 128:(dc + 1) * 128], XST[i][:, kc, :], kc == 0, kc == 7, [('WB3', i)] + xk, [pkey(b3_)])
                for dc in range(2):
                    b1_, b3_ = (0, 1) if dc == 0 else (5, 6)
                    kb.op('act', lambda e, dc=dc, b1_=b1_: e.activation(out=SG[:, dc, :], in_=PS[b1_][:, 0:RB], func=AF.Silu), [pkey(b1_)], [('SG', dc)])
                    kb.op('dve', lambda e, dc=dc, b3_=b3_: e.tensor_tensor(out=HT[:, dc, :], in0=SG[:, dc, :], in1=PS[b3_][:, 0:RB], op=ALU.mult), [('SG', dc), pkey(b3_)], [('HT', dc)])

            def slot_Y(sl):
                i = sl % 2
                W2 = WB[i][:, 4096:6144].rearrange("p (j c) -> p j c", c=1024)
                for r in range(NRT):
                    for half in range(2):
                        pq[0] += 1
                        b = 3 + pq[0] % 2
                        for dc in range(2):
                            mm(PS[b][:, 0:512], HT[:, dc, r * 128:(r + 1) * 128], W2[:, dc, half * 512:(half + 1) * 512], dc == 0, dc == 1,
                               [('HT', dc), ('WB2a', i), ('WB2b', i)], [pkey(b)])
                        copy_op('act' if half else 'dve', YS[i][:, r, half * 512:(half + 1) * 512], PS[b][:, 0:512], [pkey(b)], [('YS', i)])
                kb.dma('sp', 'yso%d' % i, lambda e: e.dma_start(out=ys_d[sl * RB:(sl + 1) * RB, :].rearrange("(r p) d -> p r d", p=128), in_=YS[i]),
                       reads=[('YS', i)], writes=[('ys', sl)])

            slot_w(0)
            slot_x(0)
            slot_x(1)
            slot_T(0)
            for sl in range(NSLOT):
                if sl + 1 < NSLOT:
                    slot_w(sl + 1)
                if sl + 2 < NSLOT:
                    slot_x(sl + 2)
                slot_cast(sl)
                slot_H(sl)
                if sl + 1 < NSLOT:
                    slot_T(sl + 1)
                slot_Y(sl)
            kb.barrier()
            G0 = [sb("G0_%d" % i, [128, D], BF16) for i in range(2)]
            A0 = [sb("A0_%d" % i, [128, D], F32) for i in range(2)]
            YO = [sb("YO_%d" % i, [128, D], F32) for i in range(2)]
            G1g = [sb("G1_%d" % i, [128, D], BF16) for i in range(2)]
            lns = [(sb("mst%d" % i, [128, 12], F32), sb("mmv%d" % i, [128, 2], F32), sb("mrs%d" % i, [128, 1], F32),
                    sb("mnm%d" % i, [128, 1], F32)) for i in range(3)]
            def comb_loads(t):
                i, x3 = t % 2, t % 3
                kb.dma('sp', 'cmx%d' % x3, lambda e: e.dma_start(out=XT[x3], in_=src_d[t * 128:(t + 1) * 128, :]),
                       reads=[('xb', t)], writes=[('MX', x3)])
                kb.dma('pool', 'cg0_%d' % i, lambda e: e.indirect_dma_start(out=G0[i], out_offset=None, in_=ys_d,
                       in_offset=bass.IndirectOffsetOnAxis(ap=DEST[:, 0, t:t + 1], axis=0)), reads=['DEST'], writes=[('G0', i)])
                kb.dma('pool', 'cg1_%d' % i, lambda e: e.indirect_dma_start(out=G1g[i], out_offset=None, in_=ys_d,
                       in_offset=bass.IndirectOffsetOnAxis(ap=DEST[:, 1, t:t + 1], axis=0)), reads=['DEST'], writes=[('G1g', i)])

            XBc1 = [sb("cbxb%d" % i, [128, D], BF16) for i in range(2)]
            XTs1 = [sb("cbxs%d" % i, [128, 8, 128], BF16) for i in range(2)]

            def cb_fin(tt):
                ii, x3 = tt % 2, tt % 3
                ln_apply(lns[x3], XT[x3], ('MX', x3), YO[ii], ('YO', ii), g2, b2, 'moep', 'ln2_%d' % x3, gain_engine='dve' if tt % 2 else 'pool')
                kb.dma('sp', 'mo%d' % ii, lambda e: e.dma_start(out=dst_d[tt * 128:(tt + 1) * 128, :], in_=YO[ii]),
                       reads=[('YO', ii)], writes=[('xa', tt)])
            comb_loads(0)
            for t in range(NT):
                i, x3 = t % 2, t % 3
                if t + 1 < NT:
                    comb_loads(t + 1)
                kb.op('act', lambda e, i=i, t=t: e.activation(out=A0[i], in_=G0[i], func=AF.Identity, scale=GATE[0][:, t:t + 1]),
                      [('G0', i), 'gate0'], [('A0', i)])
                kb.op('dve', lambda e, i=i, x3=x3: e.scalar_tensor_tensor(out=XT[x3], in0=XT[x3], scalar=ALPHA, in1=A0[i], op0=ALU.mult, op1=ALU.add),
                      [('MX', x3), ('A0', i)], [('MX', x3)])
                kb.op('dve', lambda e, i=i, t=t, x3=x3: e.scalar_tensor_tensor(out=XT[x3], in0=G1g[i], scalar=GATE[1][:, t:t + 1], in1=XT[x3], op0=ALU.mult, op1=ALU.add),
                      [('MX', x3), ('G1g', i), 'gate1'], [('MX', x3)])
                ln_stats(lns[x3], XT[x3], ('MX', x3), 'ln2_%d' % x3)
                if t >= 1:
                    cb_fin(t - 1)
                if t >= 2 and l + 1 < nlayers:
                    emit_xt_tile(YO[t % 2], ('YO', t % 2), t - 2, XBc1, XTs1, t % 2)
            cb_fin(NT - 1)
            if l + 1 < nlayers:
                emit_xt_tile(YO[NT % 2], ('YO', NT % 2), NT - 2, XBc1, XTs1, 0)
                emit_xt_tile(YO[(NT - 1) % 2], ('YO', (NT - 1) % 2), NT - 1, XBc1, XTs1, 1)
        kb.barrier()

    def copy_stream(src_d, dst_d):
        with ExitStack() as es:
            sb = mk_sb(es)
            Cb = [sb("cpy%d" % i, [128, 4, D], F32) for i in range(2)]
            for t in range(NT // 4):
                i = t % 2
                kb.dma('sp', 'cpi%d' % i, lambda e, i=i, t=t: e.dma_start(out=Cb[i], in_=src_d[t * 512:(t + 1) * 512, :].rearrange("(r p) d -> p r d", p=128)), writes=[('cpy', i)])
                kb.dma('sp', 'cpo%d' % i, lambda e, i=i, t=t: e.dma_start(out=dst_d[t * 512:(t + 1) * 512, :].rearrange("(r p) d -> p r d", p=128), in_=Cb[i]), reads=[('cpy', i)])
        kb.barrier()

    if phases is None:
        phases = ['ln0'] + sum([['mix%d' % l, 'moe%d' % l] for l in range(nlayers)], [])
    kb.barrier()
    cur = None
    for ph in phases:
        if ph == 'ln0':
            phase_ln0(xa_d); cur = xa_d
        elif ph == 'in_xa':
            copy_stream(dr['x'], xa_d); cur = xa_d
        elif ph == 'in_xb':
            copy_stream(dr['x'], xb_d); cur = xb_d
        elif ph.startswith('mix'):
            phase_mix(int(ph[3:]), xa_d, xb_d); cur = xb_d
        elif ph.startswith('moe'):
            last = (ph == phases[-1])
            phase_moe(int(ph[3:]), xb_d, y_d if last else xa_d); cur = y_d if last else xa_d
    if cur is not y_d:
        copy_stream(cur, y_d)
    kb.finish()
    es_global.close()
    return nc, kb


def _prep_shared(inp):
    f = lambda a: np.ascontiguousarray(np.asarray(a, dtype=np.float32))
    rep = lambda v: f(np.broadcast_to(np.asarray(v)[None, :], (128, np.asarray(v).shape[-1])))
    repl = lambda v: f(np.broadcast_to(np.asarray(v)[:, None, :], (L, 128, np.asarray(v).shape[-1])))
    sh = {}
    sh['ln_in_g'] = rep(inp['ln_in_g']); sh['ln_in_b'] = rep(inp['ln_in_b'])
    sh['w_in'] = f(inp['w_in'])
    sh['nb_f'] = f(np.asarray(inp['b_f']).reshape(L, 4, 1))
    sh['conv_w'] = f(np.asarray(inp['conv_w']).reshape(L, 3, 2, 128).transpose(0, 3, 2, 1))
    sh['conv_b'] = f(np.asarray(inp['conv_b']).reshape(L, 2, 128).transpose(0, 2, 1))
    sh['sgu_ln_g'] = repl(inp['sgu_ln_g']); sh['sgu_ln_b'] = repl(inp['sgu_ln_b'])
    sh['sgu_w'] = f(inp['sgu_w'])
    sh['sgu_b'] = f(np.asarray(inp['sgu_b']).reshape(L, 2, 2, 128))
    sh['grp_g'] = f(np.asarray(inp['grp_g']).reshape(L, 8, 128).transpose(0, 2, 1))
    sh['w_out'] = f(inp['w_out'])
    sh['ln1_g'] = repl(inp['ln1_g']); sh['ln1_b'] = repl(inp['ln1_b'])
    sh['router_w'] = f(np.concatenate([np.asarray(inp['router_g_w']), np.asarray(inp['router_e_w'])], axis=-1))
    sh['router_b'] = repl(np.concatenate([np.asarray(inp['router_g_b']), np.asarray(inp['router_e_b'])], axis=-1))
    w1 = np.asarray(inp['w1']).reshape(L, NE, 8, 128, 256).transpose(0, 1, 3, 2, 4).reshape(L, NE, 128, 2048)
    w3 = np.asarray(inp['w3']).reshape(L, NE, 8, 128, 256).transpose(0, 1, 3, 2, 4).reshape(L, NE, 128, 2048)
    w2 = np.asarray(inp['w2']).reshape(L, NE, 2, 128, 1024).transpose(0, 1, 3, 2, 4).reshape(L, NE, 128, 2048)
    sh['wexp'] = f(np.concatenate([w1, w3, w2], axis=-1).reshape(L * NE * 128, 6144))
    sh['ln2_g'] = repl(inp['ln2_g']); sh['ln2_b'] = repl(inp['ln2_b'])
    sh.update(_consts())
    return sh


_CACHE = {}


def kernel(**inputs):
    x = np.asarray(inputs['x'], dtype=np.float32)
    sh = _prep_shared(inputs)
    if 'nc' not in _CACHE:
        _CACHE['nc'] = build_program()[0]
    nc = _CACHE['nc']
    in_maps = []
    for c in range(NCORES):
        m = dict(sh)
        m['x'] = np.ascontiguousarray(x[c * NSEQ:(c + 1) * NSEQ].reshape(T, D))
        in_maps.append(m)
    res = run_bass_kernel_spmd(nc, in_maps, core_ids=list(range(NCORES)))
    out = np.stack([np.asarray(r['y']).reshape(NSEQ, S, D) for r in res.results], axis=0)
    return out.reshape(NCORES * NSEQ, S, D).astype(np.float32)
```

```python
import math
from contextlib import ExitStack
import numpy as np
import ml_dtypes
import concourse.bass as bass
import concourse.mybir as mybir
from concourse.bass_utils import run_bass_kernel_spmd

F32 = mybir.dt.float32
BF16 = mybir.dt.bfloat16
I32 = mybir.dt.int32
AF = mybir.ActivationFunctionType
ALU = mybir.AluOpType
AX = mybir.AxisListType

NCORES = 8
L = 2
D = 1024
S = 2048
NSEQ = 2
T = NSEQ * S
NT = T // 128
PW = 2820
NE = 32
RB = 512
NSLOT = 47
NRT = RB // 128
PROWS = NSLOT * RB
ALPHA = (2.0 * L) ** 0.25
LN_EPS = 1e-5
RMS_EPS = 1e-6
DEBUG_ALLOC = False
PE_WARM_A = 0
PE_WARM_B = 0


class KB:
    ENG = ('pe', 'act', 'dve', 'pool', 'sp')
    EPOCH = 30000

    def __init__(self, nc):
        self.nc = nc
        self.e = {'pe': nc.tensor, 'act': nc.scalar, 'dve': nc.vector, 'pool': nc.gpsimd, 'sp': nc.sync}
        self.semh = {}
        self.cur = {}
        self.cnt = {}
        self.ep = {k: -1 for k in self.ENG}
        for k in self.ENG:
            self._new_epoch(k)
        self.seen = {k: {} for k in self.ENG}
        self.last_w = {}
        self.readers = {}
        self.tokclock = {}
        self.nwaits = 0
        self.ninstr = {k: 0 for k in self.ENG}
        self.lazy = set()

    def _new_epoch(self, k):
        self.ep[k] += 1
        name = '%s#%d' % (k, self.ep[k])
        self.semh[name] = self.nc.alloc_semaphore('s_%s_%d' % (k, self.ep[k]))
        self.cur[k] = name
        self.cnt[name] = 0

    def _group(self, g):
        name = 'd:' + g
        if name not in self.semh:
            self.semh[name] = self.nc.alloc_semaphore('sd_' + g)
            self.cnt[name] = 0
        return name

    def _deps(self, reads, writes):
        deps = {}

        def add(s, v):
            if deps.get(s, 0) < v:
                deps[s] = v
        for k in reads:
            t = self.last_w.get(k)
            if t is not None:
                add(*t)
        for k in writes:
            t = self.last_w.get(k)
            if t is not None:
                add(*t)
            for s, v in self.readers.get(k, {}).items():
                add(s, v)
        return deps

    def _wait(self, engine, deps):
        eng = self.e[engine]
        seen = self.seen[engine]
        for s, v in deps.items():
            if engine == 'pe' and s.startswith('pe#'):
                continue
            if seen.get(s, 0) >= v:
                continue
            eng.wait_ge(self.semh[s], v)
            self.nwaits += 1
            for s2, v2 in self.tokclock.get((s, v), {}).items():
                if seen.get(s2, 0) < v2:
                    seen[s2] = v2
            seen[s] = v

    def _record(self, tok, reads, writes):
        for k in writes:
            self.last_w[k] = tok
            self.readers[k] = {}
        for k in reads:
            r = self.readers.setdefault(k, {})
            if r.get(tok[0], 0) < tok[1]:
                r[tok[0]] = tok[1]

    def op(self, engine, fn, reads=(), writes=()):
        self._wait(engine, self._deps(reads, writes))
        ins = fn(self.e[engine])
        name = self.cur[engine]
        self.cnt[name] += 1
        ins.then_inc(self.semh[name], 1)
        tok = (name, self.cnt[name])
        ck = dict(self.seen[engine])
        ck[name] = self.cnt[name]
        self.tokclock[tok] = ck
        self._record(tok, reads, writes)
        self.ninstr[engine] += 1
        if self.cnt[name] >= self.EPOCH:
            self._new_epoch(engine)
        return tok

    def dma(self, queue, group, fn, reads=(), writes=()):
        self._wait(queue, self._deps(reads, writes))
        ins = fn(self.e[queue])
        name = self._group(group)
        self.cnt[name] += 16
        ins.then_inc(self.semh[name], 16)
        tok = (name, self.cnt[name])
        self.tokclock[tok] = dict(self.seen[queue])
        self._record(tok, reads, writes)
        self.ninstr[queue] += 1
        return tok

    def barrier(self, full=False):
        for k in self.ENG:
            eng = self.e[k]
            for name, h in self.semh.items():
                if name in self.lazy and not full:
                    continue
                v = self.cnt[name]
                if v > 0 and self.seen[k].get(name, 0) < v:
                    eng.wait_ge(h, v)
                    self.seen[k][name] = v
        self.last_w = {}
        self.readers = {}
        self.tokclock = {}

    def finish(self):
        sp = self.e['sp']
        for name, h in self.semh.items():
            if self.cnt[name] > 0:
                sp.wait_ge(h, self.cnt[name])


def _consts():
    bf = ml_dtypes.bfloat16
    j = np.arange(128)[:, None]
    t = np.arange(128)[None, :]
    c = {}
    c['c_ident'] = np.eye(128, dtype=np.float32).astype(bf)
    c['c_identf'] = np.eye(128, dtype=np.float32)
    c['c_mstrict'] = (j < t).astype(np.float32).astype(bf)
    c['c_mincl'] = (j <= t).astype(np.float32).astype(bf)
    c['c_n8tri'] = (-8.0 * (j >= t)).astype(np.float32).astype(bf)
    c['c_negA'] = (-30000.0 * (j >= t)).astype(np.float32).astype(bf)
    c['c_negB'] = (-30000.0 * (j > t)).astype(np.float32).astype(bf)
    c['c_trirank'] = (j < t).astype(np.float32).astype(bf)
    cid = np.arange(128) // 64
    c['c_sgumask'] = (cid[None, :] <= cid[:, None]).astype(np.float32)
    sel = np.zeros((128, 8, 6), np.float32)
    for h in range(4):
        sel[h, h, 0] = 1; sel[32 + h, h, 1] = 1; sel[64 + h, h, 2] = 1
        sel[96, h, 3] = 1; sel[96, h, 4] = 1; sel[96, h, 5] = 1
        sel[96, 4 + h, 0] = 1; sel[96, 4 + h, 1] = 1; sel[96, 4 + h, 2] = 1
        sel[h, 4 + h, 3] = -1; sel[32 + h, 4 + h, 4] = -1; sel[64 + h, 4 + h, 5] = -1
    c['c_sel'] = sel.astype(bf)
    selh = np.zeros((2, 128), np.float32); selh[0, :64] = 1; selh[1, 64:] = 1
    c['c_selh'] = selh
    selden = np.zeros((65, 64), np.float32); selden[64, :] = 1
    c['c_selden'] = selden
    c['c_slotv'] = np.broadcast_to((np.arange(NSLOT, dtype=np.float32) * RB)[None, :], (128, NSLOT)).copy()
    c['c_pidx'] = np.arange(128, dtype=np.float32).reshape(128, 1)
    return c


CONST_SPECS = {
    'c_ident': ([128, 128], BF16), 'c_identf': ([128, 128], F32), 'c_mstrict': ([128, 128], BF16),
    'c_mincl': ([128, 128], BF16), 'c_n8tri': ([128, 128], BF16), 'c_negA': ([128, 128], BF16), 'c_negB': ([128, 128], BF16), 'c_trirank': ([128, 128], BF16),
    'c_sgumask': ([128, 128], F32), 'c_sel': ([128, 8, 6], BF16), 'c_selh': ([2, 128], F32),
    'c_selden': ([65, 64], F32), 'c_slotv': ([128, NSLOT], F32), 'c_pidx': ([128, 1], F32),
}

PARAM_SPECS = {
    'x': [T, D],
    'ln_in_g': [128, D], 'ln_in_b': [128, D],
    'w_in': [L, D, PW],
    'nb_f': [L, 4, 1],
    'conv_w': [L, 128, 2, 3], 'conv_b': [L, 128, 2],
    'sgu_ln_g': [L, 128, 256], 'sgu_ln_b': [L, 128, 256],
    'sgu_w': [L, 4, 128, 128], 'sgu_b': [L, 2, 2, 128],
    'grp_g': [L, 128, 8],
    'w_out': [L, D, D],
    'ln1_g': [L, 128, D], 'ln1_b': [L, 128, D],
    'router_w': [L, D, 36], 'router_b': [L, 128, 36],
    'wexp': [L * NE * 128, 6144],
    'ln2_g': [L, 128, D], 'ln2_b': [L, 128, D],
}


def build_program(phases=None, nlayers=L):
    nc = bass.Bass("TRN2", target_bir_lowering=False)
    kb = KB(nc)
    dr = {}
    for name, shape in PARAM_SPECS.items():
        dr[name] = nc.dram_tensor(name, shape, F32, kind="ExternalInput").ap()
    for name, (shape, dt) in CONST_SPECS.items():
        dr[name] = nc.dram_tensor(name, shape, dt, kind="ExternalInput").ap()
    y_d = nc.dram_tensor("y", [T, D], F32, kind="ExternalOutput").ap()
    xa_d = nc.dram_tensor("xa", [T, D], F32, kind="Internal").ap()
    xb_d = nc.dram_tensor("xb", [T, D], F32, kind="Internal").ap()
    xs_d = nc.dram_tensor("xs", [PROWS, D], BF16, kind="Internal").ap()
    ys_d = nc.dram_tensor("ys", [PROWS, D], BF16, kind="Internal").ap()
    lg_d = nc.dram_tensor("lgd", [NT, 128, 36], F32, kind="Internal").ap()
    xt_d = nc.dram_tensor("xtd", [NSEQ, 128, 8, S], BF16, kind="Internal").ap()

    es_global = ExitStack()
    wexp_bound = nc.gpsimd.to_reg(L * NE * 128 - 1)
    uid = [0]

    def mk_sb(es):
        def sb(name, shape, dt):
            uid[0] += 1
            r = es.enter_context(nc.sbuf_tensor("%s_%d" % (name, uid[0]), shape, dt)).ap()
            if DEBUG_ALLOC:
                print('alloc', name, shape, dt, 'remaining', nc.sbuf_bytes_remaining)
            return r
        return sb
    gsb = mk_sb(es_global)
    PS = [es_global.enter_context(nc.psum_tensor("ps%d" % i, [128, 512], F32)).ap() for i in range(8)]

    def pkey(i):
        return ('ps', i)

    ident = gsb("ident", [128, 128], BF16)
    identf = gsb("identf", [128, 128], F32)
    mstrict = gsb("mstrict", [128, 128], BF16)
    mincl = gsb("mincl", [128, 128], BF16)
    n8tri = gsb("n8tri", [128, 128], BF16)
    negA = gsb("negA", [128, 128], BF16)
    negB = gsb("negB", [128, 128], BF16)
    n8ones = gsb("n8ones", [128, 128], BF16)
    trirank = gsb("trirank", [128, 128], BF16)
    onesb = gsb("onesb", [128, 128], BF16)
    ones256 = gsb("ones256", [128, 128], BF16)
    csel = gsb("csel", [128, 8, 6], BF16)
    epsln = gsb("epsln", [128, 1], F32)
    epsrms = gsb("epsrms", [128, 1], F32)
    for nm, ap in (('c_ident', ident), ('c_identf', identf), ('c_mstrict', mstrict), ('c_mincl', mincl),
                   ('c_n8tri', n8tri), ('c_trirank', trirank), ('c_sel', csel), ('c_negA', negA), ('c_negB', negB)):
        kb.dma('sp', 'const', lambda e, ap=ap, nm=nm: e.dma_start(out=ap, in_=dr[nm]), writes=[nm])
    zrow = gsb("zrow", [128, D], BF16)
    kb.op('pool', lambda e: e.memset(zrow, 0.0), writes=['zrow'])
    kb.lazy.add('d:fill')

    def fill_xs_pieces():
        def mk(r0):
            return lambda: kb.dma('sp', 'fill', lambda e: e.dma_start(out=xs_d[r0:r0 + 128, :], in_=zrow), reads=['zrow'], writes=[('xsfill', r0)])
        return [mk(r0) for r0 in range(0, PROWS, 128)]

    kb.op('pool', lambda e: e.memset(n8ones, -8.0), writes=['n8ones'])
    kb.op('pool', lambda e: e.memset(onesb, 1.0), writes=['onesb'])
    kb.op('pool', lambda e: e.memset(ones256, 1.0 / 256.0), writes=['ones256'])
    kb.op('pool', lambda e: e.memset(epsln, LN_EPS), writes=['epsln'])
    kb.op('pool', lambda e: e.memset(epsrms, RMS_EPS), writes=['epsrms'])
    CK = ['c_ident', 'c_identf', 'c_mstrict', 'c_mincl', 'c_n8tri', 'c_trirank', 'c_sel',
          'n8ones', 'onesb', 'ones256', 'epsln', 'epsrms']

    rr = {'ev': 0}

    def evac_engine():
        rr['ev'] += 1
        return 'act' if rr['ev'] % 2 else 'dve'

    def copy_op(engine, out, in_, reads, writes):
        if engine == 'act':
            kb.op('act', lambda e: e.activation(out=out, in_=in_, func=AF.Copy), reads, writes)
        else:
            kb.op(engine, lambda e: e.tensor_copy(out=out, in_=in_), reads, writes)

    def mm(out, lhsT, rhs, start, stop, reads, writes):
        kb.op('pe', lambda e: e.matmul(out, lhsT=lhsT, rhs=rhs, start=start, stop=stop, skip_group_check=True),
              reads, writes)

    def ln_stats(sbs, R, Rk, tag):
        st, mv, rs, nm = sbs
        kb.op('dve', lambda e: e.bn_stats(out=st[:, 0:6], in_=R[:, 0:512]), [Rk], [tag + 'st'])
        kb.op('dve', lambda e: e.bn_stats(out=st[:, 6:12], in_=R[:, 512:1024]), [Rk], [tag + 'st'])
        kb.op('dve', lambda e: e.bn_aggr(out=mv, in_=st), [tag + 'st'], [tag + 'mv'])
        kb.op('act', lambda e: e.activation(out=rs, in_=mv[:, 1:2], func=AF.Sqrt, bias=epsln, scale=1.0),
              [tag + 'mv', 'epsln'], [tag + 'rs'])
        kb.op('dve', lambda e: e.reciprocal(out=rs, in_=rs), [tag + 'rs'], [tag + 'rs'])
        kb.op('dve', lambda e: e.scalar_tensor_tensor(out=nm, in0=mv[:, 0:1], scalar=-1.0, in1=rs,
                                                        op0=ALU.mult, op1=ALU.mult), [tag + 'mv', tag + 'rs'], [tag + 'nm'])

    def ln_apply(sbs, R, Rk, Y, Yk, g_bc, b_bc, gk, tag, gain_engine='pool'):
        st, mv, rs, nm = sbs
        kb.op('act', lambda e: e.activation(out=Y, in_=R, func=AF.Identity, scale=rs, bias=nm),
              [Rk, tag + 'rs', tag + 'nm'], [Yk])
        kb.op(gain_engine, lambda e: e.tensor_tensor(out=Y, in0=Y, in1=g_bc, op=ALU.mult), [Yk, gk], [Yk])
        kb.op('pool' if gain_engine == 'dve' else 'dve', lambda e: e.tensor_tensor(out=Y, in0=Y, in1=b_bc, op=ALU.add), [Yk, gk], [Yk])

    def emit_xt_tile(Y, Yk, gt, XBc, XTs, i2):
        sq, tl = gt // 16, gt % 16
        tpx = PS[2].bitcast(BF16)
        kb.op('act', lambda e: e.activation(out=XBc[i2], in_=Y, func=AF.Copy), [Yk], [('XBc', i2)])
        for kc in range(8):
            kb.op('pe', lambda e, kc=kc: e.transpose(out=tpx[:, kc * 128:(kc + 1) * 128], in_=XBc[i2][:, kc * 128:(kc + 1) * 128], identity=ident),
                  [('XBc', i2), 'c_ident'], [pkey(2)])
        kb.op('act', lambda e: e.activation(out=XTs[i2], in_=tpx.rearrange("p (k c) -> p k c", c=128), func=AF.Copy), [pkey(2)], [('XTs', i2)])
        kb.dma('sp', 'xto%d' % i2, lambda e: e.dma_start(out=xt_d[sq][:, :, tl * 128:(tl + 1) * 128], in_=XTs[i2]),
               reads=[('XTs', i2)], writes=[('xtd', gt)])

    def emit_ln(sbs, R, Rk, Y, Yk, g_bc, b_bc, gk, tag, gain_engine='pool'):
        ln_stats(sbs, R, Rk, tag)
        ln_apply(sbs, R, Rk, Y, Yk, g_bc, b_bc, gk, tag, gain_engine)

    def phase_ln0(dst_d):
        with ExitStack() as es:
            sb = mk_sb(es)
            g_bc = sb("l0g", [128, D], F32); b_bc = sb("l0b", [128, D], F32)
            kb.dma('sp', 'l0w', lambda e: e.dma_start(out=g_bc, in_=dr['ln_in_g']), writes=['l0gb'])
            kb.dma('sp', 'l0w', lambda e: e.dma_start(out=b_bc, in_=dr['ln_in_b']), writes=['l0gb'])
            kb.barrier()
            X = [sb("l0x%d" % i, [128, D], F32) for i in range(4)]
            sbs = [(sb("l0st%d" % i, [128, 12], F32), sb("l0mv%d" % i, [128, 2], F32),
                    sb("l0rs%d" % i, [128, 1], F32), sb("l0nm%d" % i, [128, 1], F32)) for i in range(4)]

            XBc0 = [sb("l0xb%d" % i, [128, D], BF16) for i in range(2)]
            XTs0 = [sb("l0xs%d" % i, [128, 8, 128], BF16) for i in range(2)]

            def l0_load(t):
                i = t % 4
                kb.dma('sp', 'l0in%d' % i, lambda e: e.dma_start(out=X[i], in_=dr['x'][t * 128:(t + 1) * 128, :]), writes=[('l0x', i)])

            def l0_fin(t):
                i = t % 4
                ln_apply(sbs[i], X[i], ('l0x', i), X[i], ('l0x', i), g_bc, b_bc, 'l0gb', 'l0_%d' % i, gain_engine='pool' if t % 2 else 'dve')
                kb.dma('sp', 'l0out%d' % i, lambda e: e.dma_start(out=dst_d[t * 128:(t + 1) * 128, :], in_=X[i]),
                       reads=[('l0x', i)], writes=[('xa', t)])
            l0_load(0)
            l0_load(1)
            for t in range(NT):
                i = t % 4
                ln_stats(sbs[i], X[i], ('l0x', i), 'l0_%d' % i)
                if t >= 1:
                    l0_fin(t - 1)
                if t >= 2:
                    emit_xt_tile(X[(t - 2) % 4], ('l0x', (t - 2) % 4), t - 2, XBc0, XTs0, t % 2)
                if t + 2 < NT:
                    l0_load(t + 2)
            l0_fin(NT - 1)
            emit_xt_tile(X[(NT - 2) % 4], ('l0x', (NT - 2) % 4), NT - 2, XBc0, XTs0, 0)
            emit_xt_tile(X[(NT - 1) % 4], ('l0x', (NT - 1) % 4), NT - 1, XBc0, XTs0, 1)
        kb.barrier()

    def phase_mix(l, src_d, dst_d):
        fill_pieces = fill_xs_pieces()
        with ExitStack() as es:
            sb = mk_sb(es)
            w_in_v = dr['w_in'][l].rearrange("(kc p) n -> p kc n", p=128)
            w_out_v = dr['w_out'][l].rearrange("(kc p) n -> p kc n", p=128)
            WOUT = sb("WOUT", [128, 8, D], BF16)
            WG = sb("WG", [128, 8, 1540], BF16)
            WST = [sb("WST%d" % i, [128, 8, 64], F32) for i in range(2)]
            sg_g = sb("sgg", [128, 256], F32); sg_b = sb("sgb", [128, 256], F32)
            cw = sb("cw", [128, 2, 3], F32); cb = sb("cb", [128, 2], F32)
            gg = sb("gg", [128, 8], F32)
            nbf = sb("nbf", [4, 1], F32)
            WRr = sb("WRr", [128, 8, 36], F32)
            RBr = sb("RBr", [128, 36], F32)
            LGT = [sb("LGT%d" % i, [128, 36], F32) for i in range(2)]
            WmT = sb("WmT", [128, 4, 128], BF16)
            BS4 = sb("BS4", [128, 2, 4, 128], F32)
            kb.dma('sp', 'mixp', lambda e: e.dma_start(out=WRr, in_=dr['router_w'][l].rearrange("(kc p) n -> p kc n", p=128)), writes=['mixp'])
            kb.dma('sp', 'mixp', lambda e: e.dma_start(out=RBr, in_=dr['router_b'][l]), writes=['mixp'])
            for ap, nm in ((sg_g, 'sgu_ln_g'), (sg_b, 'sgu_ln_b'), (cw, 'conv_w'),
                           (cb, 'conv_b'), (gg, 'grp_g'), (nbf, 'nb_f')):
                kb.dma('sp', 'mixp', lambda e, ap=ap, nm=nm: e.dma_start(out=ap, in_=dr[nm][l]), writes=['mixp'])
            wst_i = [0]

            def load_cols(dst, dst_key, src_v, c0, c1, d0=0, engines=('pool',), pieces=None):
                dkeys = dst_key if isinstance(dst_key, list) else [dst_key]
                o = 0
                while c0 + o < c1:
                    n = min(64, c1 - c0 - o)
                    i = wst_i[0] % 2
                    wst_i[0] += 1
                    def piece(i=i, o=o, n=n, eng=engines[wst_i[0] % len(engines)]):
                        kb.dma('sp', 'wst%d' % i, lambda e: e.dma_start(out=WST[i][:, :, 0:n], in_=src_v[:, :, c0 + o:c0 + o + n]),
                               writes=[('wst', i)])
                        copy_op(eng, dst[:, :, d0 + o:d0 + o + n], WST[i][:, :, 0:n], [('wst', i)], dkeys)
                    if pieces is None:
                        piece()
                    else:
                        pieces.append(piece)
                    o += n
            load_cols(WOUT, 'WOUT', w_out_v, 0, D, engines=('dve', 'act', 'pool'))
            with ExitStack() as es2:
                sb2 = mk_sb(es2)
                sgm = sb2("sgm", [128, 128], F32)
                ws = sb2("ws", [128, 4, 128], F32)
                wsb = sb2("wsb", [128, 4, 128], BF16)
                bs2 = sb2("bs2", [2, 2, 128], F32)
                selh = sb2("selh", [2, 128], F32)
                kb.dma('sp', 'mixp', lambda e: e.dma_start(out=sgm, in_=dr['c_sgumask']), writes=['sgm'])
                kb.dma('sp', 'mixp', lambda e: e.dma_start(out=selh, in_=dr['c_selh']), writes=['selh'])
                kb.dma('sp', 'mixp', lambda e: e.dma_start(out=ws, in_=dr['sgu_w'][l].rearrange("g i j -> i g j")), writes=['ws'])
                kb.dma('sp', 'mixp', lambda e: e.dma_start(out=bs2, in_=dr['sgu_b'][l].rearrange("c h i -> h c i")), writes=['bs2'])
                kb.barrier()
                kb.op('dve', lambda e: e.tensor_scalar(out=nbf, in0=nbf, scalar1=-1.0, scalar2=None, op0=ALU.mult),
                      ['mixp'], ['mixp'])
                for g in range(4):
                    kb.op('dve', lambda e, g=g: e.tensor_tensor(out=wsb[:, g, :], in0=ws[:, g, :], in1=sgm, op=ALU.mult),
                          ['ws', 'sgm'], ['wsb'])
                tpb = PS[2].bitcast(BF16)
                for g in range(4):
                    kb.op('pe', lambda e, g=g: e.transpose(out=tpb[:, g * 128:(g + 1) * 128], in_=wsb[:, g, :], identity=ident),
                          ['wsb', 'c_ident'], [pkey(2)])
                kb.op('dve', lambda e: e.tensor_copy(out=WmT.rearrange("p g i -> p (g i)"), in_=tpb[:, 0:512]), [pkey(2)], ['WmT'])
                for cc in range(2):
                    mm(PS[3][:, 0:128], selh, bs2[:, cc, :], True, True, ['selh', 'bs2'], [pkey(3)])
                    for r in range(4):
                        kb.op('dve', lambda e, cc=cc, r=r: e.tensor_copy(out=BS4[:, cc, r, :], in_=PS[3][:, 0:128]), [pkey(3)], ['BS4'])
                kb.barrier()

            xT = sb("xT", [128, 8, S], BF16)
            MIX = sb("MIX", [128, 8, S], BF16)
            QK = sb("QK", [128, 8, S], BF16)
            VV = sb("VV", [128, 16, 4, 65], BF16)
            XT32 = [sb("XT32_%d" % i, [128, D], F32) for i in range(2)]
            XTB = [sb("XTB_%d" % i, [128, D], BF16) for i in range(2)]
            SCR = sb("SCR", [128, 5124], F32)
            ET = [SCR[:, u * 512:(u + 1) * 512] for u in range(2)]
            UB = [SCR[:, 1024 + u * 256:1024 + (u + 1) * 256].bitcast(BF16) for u in range(3)]
            PT = [SCR[:, 1792 + u * 256:1792 + (u + 1) * 256].bitcast(BF16) for u in range(2)]
            UCB = [SCR[:, 2304 + u * 256:2304 + (u + 1) * 256].bitcast(BF16) for u in range(6)]
            PTB = [XTB[0][:, 0:512], XTB[0][:, 512:1024], XTB[1][:, 0:512]]
            OSB = [SCR[0:65, 3840:4352], SCR[0:65, 3840:4352]]
            REC = SCR[0:64, 4352:4864]
            HS = SCR[:, 0:512]
            ZB = [SCR[:, 512 + cc * 514:512 + (cc + 1) * 514] for cc in range(2)]
            T1 = SCR[:, 1540:2052]
            UC = SCR[:, 2052:3076].rearrange("p (c n) -> p c n", n=512)
            VG4 = SCR[:, 3076:4100].rearrange("p (t n) -> p t n", n=256)
            T2 = SCR[:, 4100:4612]
            VLN4 = SCR[:, 4612:5124].bitcast(BF16).rearrange("p (t n) -> p t n", n=256)
            selden = sb("selden", [65, 64], F32)
            CS = sb("CS", [128, S], BF16)
            FE = SCR[0:4, 0:512]; FCC = [SCR[0:4, 512:1024], SCR[0:4, 1024:1536]]; F8 = SCR[0:4, 1536:2048]
            QA4 = MIX[:, 4:8, :]

            def VAt(t):
                return XT32[t // 8].bitcast(BF16)[:, (t % 8) * 256:(t % 8 + 1) * 256].rearrange("p (h d) -> p h d", d=64)
            FR = sb("FR", [4, 512], F32); FH = sb("FH", [4, 512], BF16)
            onesF = sb("onesF", [4, 512], F32)
            st4 = sb("vst4", [128, 4, 6], F32); mv4 = sb("vmv4", [128, 4, 2], F32); rs4 = sb("vrs4", [128, 4], F32); nm4 = sb("vnm4", [128, 4], F32)
            MIXN = xT[:, 0:2, :].rearrange("p a (b c) -> p (a b) c", c=512)
            RR = [xT[:, 2 + i, :].bitcast(F32) for i in range(2)]
            SQ = [xT[:, 4, (j % 4) * 512:(j % 4 + 1) * 512] for j in range(8)]
            XTTr = xT[:, 5, :].bitcast(F32).rearrange("p (k c) -> p k c", c=128)
            RSG = [xT[:, 6 + g // 2, (g % 2) * 1024:(g % 2 + 1) * 1024].bitcast(F32) for g in range(4)]
            XR = XT32
            lns = [(sb("ost%d" % i, [128, 12], F32), sb("omv%d" % i, [128, 2], F32), sb("ors%d" % i, [128, 1], F32),
                    sb("onm%d" % i, [128, 1], F32)) for i in range(2)]
            kb.dma('sp', 'selden', lambda e: e.dma_start(out=selden, in_=dr['c_selden']), writes=['selden'])
            kb.op('pool', lambda e: e.memset(onesF, 1.0), writes=['onesF'])
            kb.op('pool', lambda e: e.memset(VV, 1.0), writes=['VV'])
            kb.op('pool', lambda e: e.memset(CS, 0.0), writes=['CS'])
            kb.op('pool', lambda e: e.memset(CS[96:97, :], 1.0), writes=['CS'])
            tpb = PS[2].bitcast(BF16)
            pp = [0]

            def nextbank(banks=(0, 1)):
                pp[0] += 1
                return banks[pp[0] % len(banks)]

            for s in range(NSEQ):
                tok0 = s * S
                for tc in range(4):
                    kb.dma('sp', 'xtl%d' % tc, lambda e, tc=tc: e.dma_start(out=xT[:, :, tc * 512:(tc + 1) * 512], in_=xt_d[s][:, :, tc * 512:(tc + 1) * 512]),
                           writes=[('xT', tc)])

                def proj_fm(col, M, tc, bank, extra=None):
                    for kc in range(8):
                        mm(PS[bank][0:M, 0:512], WG[:, kc, wb[0] + col:wb[0] + col + M], xT[:, kc, tc * 512:(tc + 1) * 512],
                           kc == 0, kc == 7, wgk[0] + [('xT', tc)], [pkey(bank)])

                def proj_tm(col, N, t, bank):
                    for kc in range(8):
                        mm(PS[bank][:, 0:N], xT[:, kc, t * 128:(t + 1) * 128], WG[:, kc, wb[0] + col:wb[0] + col + N],
                           kc == 0, kc == 7, wgk[0] + [('xT', t // 4)], [pkey(bank)])

                wgk = [['WGB']]
                wb = [768]
                if s == 0:
                    load_cols(WG, 'WGB', w_in_v, 768, 1540, d0=768, engines=('dve', 'act', 'pool'))
                    load_cols(WG, 'WGA', w_in_v, 0, 768, engines=('dve', 'act', 'pool'))
                for tc in range(4):
                    b = nextbank()
                    cs = slice(tc * 512, (tc + 1) * 512)
                    FC = FCC[tc % 2]
                    proj_fm(768, 4, tc, b)
                    kb.op('act', lambda e, b=b: e.activation(out=FE, in_=PS[b][0:4, 0:512], func=AF.Exp, bias=nbf, scale=-1.0),
                          [pkey(b), 'mixp'], ['FE'])
                    kb.op('act', lambda e: e.activation(out=FE, in_=FE, func=AF.Ln, bias=1.0, scale=1.0), ['FE'], ['FE'])
                    init = 0.0 if tc == 0 else FCC[(tc - 1) % 2][:, 511:512]
                    kb.op('dve', lambda e, FC=FC, init=init: e.tensor_tensor_scan(out=FC, data0=onesF, data1=FE, initial=init, op0=ALU.mult, op1=ALU.add),
                          ['FE', 'onesF', ('FC', (tc - 1) % 2)], [('FC', tc % 2)])
                    kb.op('dve', lambda e, FC=FC: e.tensor_scalar(out=F8, in0=FC, scalar1=-8.0, scalar2=None, op0=ALU.mult), [('FC', tc % 2)], ['F8'])
                    kb.op('dve', lambda e, cs=cs: e.tensor_copy(out=CS[0:4, cs], in_=F8), ['F8'], ['CS'])
                    kb.op('dve', lambda e, cs=cs: e.tensor_tensor(out=FR, in0=F8, in1=CS[0:4, cs], op=ALU.subtract), ['F8', 'CS'], ['FR'])
                    kb.op('dve', lambda e: e.tensor_copy(out=FH, in_=FR), ['FR'], ['FH'])
                    kb.op('dve', lambda e, cs=cs: e.tensor_copy(out=CS[32:36, cs], in_=FH), ['FH'], ['CS'])
                    kb.op('dve', lambda e: e.tensor_tensor(out=FR, in0=FR, in1=FH, op=ALU.subtract), ['FR', 'FH'], ['FR'])
                    kb.op('dve', lambda e, cs=cs: e.tensor_copy(out=CS[64:68, cs], in_=FR), ['FR'], ['CS'])
                wgk[0] = ['WGA']
                wb[0] = 0
                for tc in range(4):
                    for gi in range(4):
                        b = nextbank()
                        proj_fm(gi * 128, 128, tc, b)
                        copy_op(evac_engine(), QA4[:, gi, tc * 512:(tc + 1) * 512], PS[b][:, 0:512], [pkey(b)], ['QKA'])
                    for tl in range(4):
                        t = tc * 4 + tl
                        b = nextbank()
                        proj_tm(512, 256, t, b)
                        copy_op(evac_engine(), VAt(t), PS[b][:, 0:256].rearrange("p (h d) -> p h d", d=64), [pkey(b)], ['VA'])
                unitsA = []
                grp = 0
                for h in range(4):
                    for c in range(4):
                        k = 0
                        for Sb in range(4 * c + 3, -1, -1):
                            q0 = max(Sb * 128, c * 512)
                            unitsA.append(dict(h=h, c=c, Sb=Sb, q0=q0, N=(c + 1) * 512 - q0, loc=q0 - c * 512, diag=Sb >= 4 * c,
                                               first=(Sb == 4 * c + 3), last=(Sb == 0), k=k, g=grp))
                            k += 1
                        grp += 1
                ZBK = (3, 4, 7)

                def a_s1(j, u):
                    hp, ho = u['h'] // 2, (u['h'] % 2) * 64
                    N, loc = u['N'], u['loc']
                    zb = ZBK[j % 3]
                    zs = PS[zb][:, loc:loc + N]
                    e2, u3 = j % 2, j % 3
                    gb = 3 * (u['g'] % 2)
                    if u['first']:
                        for q in range(3):
                            kb.op('pool', lambda e, q=q: e.memset(UCB[gb + q], 0.0), writes=[('UCB', gb + q)])
                    mm(zs, QA4[ho:ho + 64, 2 + hp, u['Sb'] * 128:(u['Sb'] + 1) * 128], QA4[ho:ho + 64, hp, u['q0']:u['q0'] + N], True, True, ['QKA'], [pkey(zb)])
                    if u['diag']:
                        mm(zs[:, 0:128], ident, negA, False, True, ['c_ident', 'c_negA'], [pkey(zb)])
                    for _ in range(PE_WARM_A):
                        mm(PS[1][:, 0:512], n8ones, xT[:, 0, 0:512], True, True, ['n8ones'], [pkey(1)])
                    kb.op('act', lambda e: e.activation(out=ET[e2][:, 0:N], in_=zs, func=AF.Exp, scale=0.125), [pkey(zb)], [('ET', e2)])
                    kb.op('act', lambda e: e.activation(out=UB[u3][:, 0:N], in_=ET[e2][:, 0:N], func=AF.Ln, bias=1.0, scale=1.0), [('ET', e2)], [('UB', u3)])
                    if not u['last']:
                        kc_, kn_ = gb + u['k'] % 3, gb + (u['k'] + 1) % 3
                        kb.op('pool', lambda e: e.tensor_tensor(out=UCB[kn_][:, loc:512], in0=UCB[kc_][:, loc:512], in1=UB[u3][:, 0:N], op=ALU.add),
                              [('UCB', kc_), ('UB', u3)], [('UCB', kn_)])

                def a_s2(j, u):
                    N, loc = u['N'], u['loc']
                    zb = ZBK[j % 3]
                    zs = PS[zb][:, loc:loc + N]
                    u3, p2 = j % 3, j % 2
                    mm(zs, n8tri, UB[u3][:, 0:N], False, u['first'], [('UB', u3), 'c_n8tri'], [pkey(zb)])
                    if not u['first']:
                        kc_ = 3 * (u['g'] % 2) + u['k'] % 3
                        mm(zs, n8ones, UCB[kc_][:, loc:loc + N], False, True, [('UCB', kc_), 'n8ones'], [pkey(zb)])
                    kb.op('act', lambda e: e.activation(out=PT[p2][:, 0:N], in_=zs, func=AF.Exp, scale=0.125), [pkey(zb)], [('PT', p2)])

                def a_s3(j, u):
                    hp, ho = u['h'] // 2, (u['h'] % 2) * 64
                    N, loc = u['N'], u['loc']
                    ob = 5
                    mm(PS[ob][0:64, loc:loc + N], VAt(u['Sb'])[:, u['h'], :], PT[j % 2][:, 0:N], u['first'], u['last'], ['VA', ('PT', j % 2)], [pkey(ob)])
                    if u['last']:
                        copy_op('dve', MIX[ho:ho + 64, hp, u['c'] * 512:(u['c'] + 1) * 512], PS[ob][0:64, :], [pkey(ob)], ['MIX'])


                wgk[0] = ['WGB']
                wb[0] = 768
                for tc in range(4):
                    for gi in range(8):
                        b = nextbank()
                        proj_fm(gi * 64, 64, tc, b)
                        mm(PS[b][64:70, 0:512], csel[:, gi, :], CS[:, tc * 512:(tc + 1) * 512], True, True, ['CS', 'c_sel'], [pkey(b)])
                        copy_op(evac_engine(), QK[0:70, gi, tc * 512:(tc + 1) * 512], PS[b][0:70, 0:512], [pkey(b)], ['QK'])
                    for tl in range(4):
                        t = tc * 4 + tl
                        b = nextbank()
                        proj_tm(512, 256, t, b)
                        copy_op(evac_engine(), VV[:, t, :, 0:64], PS[b][:, 0:256].rearrange("p (h d) -> p h d", d=64), [pkey(b)], ['VV'])
                cd_pieces = []
                load_cols(WG, ['WGA', 'WGB'], w_in_v, 1540, 2820, d0=0, pieces=cd_pieces)
                unitsB = []
                grp = 0
                for h in range(4):
                    for c in range(4):
                        for Sb in range(4 * c + 3, -1, -1):
                            q0 = max(Sb * 128, c * 512)
                            unitsB.append(dict(h=h, c=c, Sb=Sb, q0=q0, N=(c + 1) * 512 - q0, loc=q0 - c * 512, diag=Sb >= 4 * c,
                                               first=(Sb == 4 * c + 3), last=(Sb == 0), g=grp))
                        grp += 1

                def b_s1(j, u):
                    h, N, loc = u['h'], u['N'], u['loc']
                    zb = (0, 1)[j % 2]
                    zs = PS[zb][:, loc:loc + N]
                    p3 = j % 3
                    mm(zs, QK[0:70, 4 + h, u['Sb'] * 128:(u['Sb'] + 1) * 128], QK[0:70, h, u['q0']:u['q0'] + N], True, True, ['QK'], [pkey(zb)])
                    if u['diag']:
                        mm(zs[:, 0:128], ident, negB, False, True, ['c_ident', 'c_negB'], [pkey(zb)])
                    for _ in range(PE_WARM_B):
                        mm(PS[1][:, 0:512], n8ones, xT[:, 0, 0:512], True, True, ['n8ones'], [pkey(1)])
                    kb.op('act', lambda e: e.activation(out=PTB[p3][:, 0:N], in_=zs, func=AF.Exp, scale=0.125), [pkey(zb)], [('PTB', p3)])

                def b_s2(j, u):
                    h, N, loc = u['h'], u['N'], u['loc']
                    hp, ho = h // 2, (h % 2) * 64
                    p3 = j % 3
                    ob = 6
                    mm(PS[ob][0:65, loc:loc + N], VV[:, u['Sb'], h, 0:65], PTB[p3][:, 0:N], u['first'], u['last'], ['VV', ('PTB', p3)], [pkey(ob)])
                    if u['last']:
                        o2 = 0
                        copy_op('act', OSB[o2], PS[ob][0:65, :], [pkey(ob)], [('OS', o2)])
                        mm(PS[2][0:64, :], selden, OSB[o2], True, True, [('OS', o2), 'selden'], [pkey(2)])
                        kb.op('dve', lambda e: e.reciprocal(out=REC, in_=PS[2][0:64, :]), [pkey(2)], ['REC'])
                        kb.op('dve', lambda e: e.tensor_tensor(out=MIX[ho:ho + 64, 2 + hp, u['c'] * 512:(u['c'] + 1) * 512], in0=OSB[o2][0:64, :], in1=REC, op=ALU.mult),
                              [('OS', o2), 'REC'], ['MIX'])

                nA, nB = len(unitsA), len(unitsB)
                for j in range(max(nA, nB) + 2):
                    if j < nA:
                        a_s1(j, unitsA[j])
                    if j < nB:
                        b_s1(j, unitsB[j])
                    if 0 <= j - 1 < nA:
                        a_s2(j - 1, unitsA[j - 1])
                    if 0 <= j - 1 < nB:
                        b_s2(j - 1, unitsB[j - 1])
                    if 0 <= j - 2 < nA:
                        a_s3(j - 2, unitsA[j - 2])
                    if j >= 2 and cd_pieces:
                        cd_pieces.pop(0)()
                    for _ in range(2):
                        if fill_pieces:
                            fill_pieces.pop(0)()
                while cd_pieces:
                    cd_pieces.pop(0)()

                kb.barrier()
                wgk[0] = ['WGA', 'WGB']
                wb[0] = 0
                for cc in range(2):
                    kb.op('pool', lambda e, cc=cc: e.memset(ZB[cc][:, 0:2], 0.0), writes=[('ZB', cc)])
                for tc in range(4):
                    cs = slice(tc * 512, (tc + 1) * 512)
                    for cc in range(2):
                        bh = nextbank((0, 1, 3, 4))
                        proj_fm(cc * 128, 128, tc, bh)
                        copy_op('act', HS, PS[bh][:, 0:512], [pkey(bh)], ['HS'])
                        bc_ = nextbank((0, 1, 3, 4))
                        proj_fm(512 + cc * 128, 128, tc, bc_)
                        kb.op('dve', lambda e, cc=cc, bc_=bc_: e.tensor_tensor(out=ZB[cc][:, 2:514], in0=HS, in1=PS[bc_][:, 0:512], op=ALU.mult),
                              ['HS', pkey(bc_)], [('ZB', cc)])
                        bb = nextbank((0, 1, 3, 4))
                        proj_fm(256 + cc * 128, 128, tc, bb)
                        kb.op('dve', lambda e, cc=cc: e.tensor_scalar(out=T1, in0=ZB[cc][:, 0:512], scalar1=cw[:, cc, 0:1], scalar2=None, op0=ALU.mult),
                              [('ZB', cc), 'mixp'], ['T1'])
                        kb.op('dve', lambda e, cc=cc: e.scalar_tensor_tensor(out=T1, in0=ZB[cc][:, 1:513], scalar=cw[:, cc, 1:2], in1=T1, op0=ALU.mult, op1=ALU.add),
                              [('ZB', cc), 'mixp', 'T1'], ['T1'])
                        kb.op('dve', lambda e, cc=cc: e.scalar_tensor_tensor(out=T1, in0=ZB[cc][:, 2:514], scalar=cw[:, cc, 2:3], in1=T1, op0=ALU.mult, op1=ALU.add),
                              [('ZB', cc), 'mixp', 'T1'], ['T1'])
                        kb.op('dve', lambda e, cc=cc, bb=bb, cs=cs: e.scalar_tensor_tensor(out=MIX[:, 4 + cc, cs], in0=T1, scalar=cb[:, cc:cc + 1], in1=PS[bb][:, 0:512], op0=ALU.add, op1=ALU.mult),
                              ['T1', 'mixp', pkey(bb)], ['MIX'])
                        kb.op('pool', lambda e, cc=cc: e.tensor_copy(out=ZB[cc][:, 0:2], in_=ZB[cc][:, 512:514]), [('ZB', cc)], [('ZB', cc)])
                    for cc in range(2):
                        bu = nextbank((0, 1, 3, 4))
                        proj_fm(768 + cc * 128, 128, tc, bu)
                        kb.op('act', lambda e, cc=cc, bu=bu: e.activation(out=UC[:, cc, :], in_=PS[bu][:, 0:512], func=AF.Gelu_apprx_tanh),
                              [pkey(bu)], ['UC'])
                    for tl in range(4):
                        t = tc * 4 + tl
                        bv = nextbank((0, 1, 3, 4))
                        proj_tm(1024, 256, t, bv)
                        kb.op('act', lambda e, bv=bv, tl=tl: e.activation(out=VG4[:, tl, :], in_=PS[bv][:, 0:256], func=AF.Gelu_apprx_tanh), [pkey(bv)], [('VG', tl)])
                        kb.op('dve', lambda e, tl=tl: e.bn_stats(out=st4[:, tl, :], in_=VG4[:, tl, :]), [('VG', tl)], [('vst', tl)])
                        kb.op('dve', lambda e, tl=tl: e.bn_aggr(out=mv4[:, tl, :], in_=st4[:, tl, :]), [('vst', tl)], [('vmv', tl)])
                    vmk = [('vmv', tl) for tl in range(4)]
                    kb.op('act', lambda e: e.activation(out=rs4, in_=mv4[:, :, 1], func=AF.Sqrt, bias=epsln, scale=1.0), vmk + ['epsln'], ['vrs'])
                    kb.op('dve', lambda e: e.reciprocal(out=rs4, in_=rs4), ['vrs'], ['vrs'])
                    kb.op('dve', lambda e: e.scalar_tensor_tensor(out=nm4, in0=mv4[:, :, 0], scalar=-1.0, in1=rs4, op0=ALU.mult, op1=ALU.mult), vmk + ['vrs'], ['vnm'])
                    for tl in range(4):
                        kb.op('act', lambda e, tl=tl: e.activation(out=VG4[:, tl, :], in_=VG4[:, tl, :], func=AF.Identity, scale=rs4[:, tl:tl + 1], bias=nm4[:, tl:tl + 1]),
                              [('VG', tl), 'vrs', 'vnm'], [('VG', tl)])
                        kb.op('pool', lambda e, tl=tl: e.tensor_tensor(out=VG4[:, tl, :], in0=VG4[:, tl, :], in1=sg_g, op=ALU.mult), [('VG', tl), 'mixp'], [('VG', tl)])
                        kb.op('dve', lambda e, tl=tl: e.tensor_tensor(out=VLN4[:, tl, :], in0=VG4[:, tl, :], in1=sg_b, op=ALU.add), [('VG', tl), 'mixp'], [('VLN', tl)])
                        for g in range(4):
                            bk = 5 + g // 2
                            mm(PS[bk][(g % 2) * 64:(g % 2) * 64 + 64, tl * 128:(tl + 1) * 128], VLN4[:, tl, g * 64:(g + 1) * 64], WmT[:, g, :],
                               True, True, [('VLN', tl), 'WmT'], [pkey(bk)])
                    for cc in range(2):
                        kb.op('dve', lambda e, cc=cc: e.tensor_tensor(out=T2, in0=PS[5 + cc][:, 0:512], in1=BS4[:, cc, :, :].rearrange("p r i -> p (r i)"), op=ALU.add),
                              [pkey(5 + cc), 'BS4'], ['T2'])
                        kb.op('pool', lambda e, cc=cc, cs=cs: e.tensor_tensor(out=MIX[:, 6 + cc, cs], in0=T2, in1=UC[:, cc, :], op=ALU.mult),
                              ['T2', 'UC'], ['MIX'])

                kb.barrier()
                g1 = SCR[:, 0:1024]
                b1 = SCR[:, 1024:2048]
                kb.dma('sp', 'g1b1', lambda e: e.dma_start(out=g1, in_=dr['ln1_g'][l]), writes=['g1b1'])
                kb.dma('sp', 'g1b1', lambda e: e.dma_start(out=b1, in_=dr['ln1_b'][l]), writes=['g1b1'])
                if s + 1 < NSEQ:
                    load_cols(WG, 'WGB', w_in_v, 768, 1540, d0=768)
                    load_cols(WG, 'WGA', w_in_v, 0, 768)
                for tc in range(4):
                    cs = slice(tc * 512, (tc + 1) * 512)
                    GB = (0, 1, 5, 6)
                    for grp in range(4):
                        b = GB[grp]
                        for j in range(2):
                            q = 2 * grp + j
                            kb.op('act', lambda e, q=q, cs=cs: e.activation(out=SQ[q], in_=MIX[:, q, cs], func=AF.Square),
                                  ['MIX'], [('SQ', q % 4)])
                            mm(PS[b][:, 0:512], ones256, SQ[q], j == 0, j == 1, [('SQ', q % 4), 'ones256'], [pkey(b)])
                    for grp in range(4):
                        b = GB[grp]
                        kb.op('act', lambda e, b=b, grp=grp: e.activation(out=RSG[grp], in_=PS[b][:, 0:512], func=AF.Sqrt, bias=epsrms, scale=1.0),
                              [pkey(b), 'epsrms'], [('RS', grp)])
                    for grp in range(4):
                        kb.op('dve', lambda e, grp=grp: e.reciprocal(out=RSG[grp], in_=RSG[grp]), [('RS', grp)], [('RS', grp)])
                        for j in range(2):
                            kc = 2 * grp + j
                            kb.op('dve', lambda e, kc=kc, cs=cs, grp=grp: e.scalar_tensor_tensor(out=MIXN[:, kc, :], in0=MIX[:, kc, cs], scalar=gg[:, kc:kc + 1], in1=RSG[grp], op0=ALU.mult, op1=ALU.mult),
                                  ['MIX', 'mixp', ('RS', grp)], [('MIXN', kc)])
                    for tl in range(4):
                        t = tc * 4 + tl
                        gt = tok0 // 128 + t
                        i = t % 2

                        def xr_load(tt):
                            ii, g_ = tt % 2, tok0 // 128 + tt
                            kb.dma('sp', 'xr%d' % ii, lambda e: e.dma_start(out=XR[ii], in_=src_d[g_ * 128:(g_ + 1) * 128, :]),
                                   reads=[('xa', g_)], writes=[('xt32', ii)])
                        if t == 0:
                            xr_load(0)
                        if t + 1 < 16:
                            xr_load(t + 1)
                        def fin_apply(tt):
                            ii, g_ = tt % 2, tok0 // 128 + tt
                            ln_apply(lns[ii], RR[ii], ('RR', ii), RR[ii], ('RR', ii), g1, b1, 'g1b1', 'ln1_%d' % ii)
                            kb.dma('sp', 'x1o%d' % ii, lambda e: e.dma_start(out=dst_d[g_ * 128:(g_ + 1) * 128, :], in_=RR[ii]),
                                   reads=[('RR', ii)], writes=[('xb', g_)])

                        def fin_router(tt):
                            ii, g_ = tt % 2, tok0 // 128 + tt
                            for hf in range(2):
                                for k4 in range(4):
                                    kc = hf * 4 + k4
                                    kb.op('pe', lambda e, kc=kc, k4=k4: e.transpose(out=PS[2][:, k4 * 128:(k4 + 1) * 128], in_=RR[ii][:, kc * 128:(kc + 1) * 128], identity=identf),
                                          [('RR', ii), 'c_identf'], [pkey(2)])
                                copy_op('act', XTTr[:, hf * 4:(hf + 1) * 4, :], PS[2].rearrange("p (k c) -> p k c", c=128), [pkey(2)], [('XTTr', hf)])
                            for kc in range(8):
                                mm(PS[7][:, 0:36], XTTr[:, kc, :], WRr[:, kc, :], kc == 0, kc == 7, [('XTTr', kc // 4), 'mixp'], [pkey(7)])
                            kb.op('dve', lambda e: e.tensor_tensor(out=LGT[ii], in0=PS[7][:, 0:36], in1=RBr, op=ALU.add), [pkey(7), 'mixp'], [('LGT', ii)])
                            kb.dma('sp', 'lgo%d' % ii, lambda e: e.dma_start(out=lg_d[g_], in_=LGT[ii]), reads=[('LGT', ii)], writes=[('lg', g_)])
                        banks = []
                        for half in range(2):
                            b = nextbank((3, 4))
                            banks.append(b)
                            for kc in range(8):
                                mm(PS[b][:, 0:512], MIXN[:, kc, tl * 128:(tl + 1) * 128], WOUT[:, kc, half * 512:(half + 1) * 512],
                                   kc == 0, kc == 7, [('MIXN', kc), 'WOUT'], [pkey(b)])
                        if t >= 1:
                            fin_apply(t - 1)
                        for half in range(2):
                            b = banks[half]
                            kb.op('dve', lambda e, i=i, half=half, b=b: e.scalar_tensor_tensor(out=RR[i][:, half * 512:(half + 1) * 512], in0=XR[i][:, half * 512:(half + 1) * 512],
                                                                                               scalar=ALPHA, in1=PS[b][:, 0:512], op0=ALU.mult, op1=ALU.add),
                                  [('xt32', i), pkey(b)], [('RR', i)])
                        ln_stats(lns[i], RR[i], ('RR', i), 'ln1_%d' % i)
                        if t >= 1:
                            fin_router(t - 1)
                        if t == 15:
                            fin_apply(15)
                            fin_router(15)
                kb.barrier()
            while fill_pieces:
                fill_pieces.pop(0)()
        kb.barrier()

    def phase_moe(l, src_d, dst_d):
        kb.barrier(full=True)
        with ExitStack() as es:
            sb = mk_sb(es)
            g2 = sb("ln2g", [128, D], F32); b2 = sb("ln2b", [128, D], F32)
            XT = [sb("MX%d" % i, [128, D], F32) for i in range(3)]
            GATE = [sb("gate%d" % k, [128, NT], F32) for k in range(2)]
            DEST = sb("DEST", [128, 2, NT], I32)
            WIDX = sb("WIDX", [128, NSLOT], I32)
            es_r = ExitStack()
            sbr = mk_sb(es_r)
            slotv = sbr("slotv", [128, NSLOT], F32)
            pidx = sbr("pidx", [128, 1], F32)
            for ap, nm in ((g2, 'ln2_g'), (b2, 'ln2_b')):
                kb.dma('sp', 'moep', lambda e, ap=ap, nm=nm: e.dma_start(out=ap, in_=dr[nm][l]), writes=['moep'])
            kb.dma('sp', 'moep', lambda e: e.dma_start(out=slotv, in_=dr['c_slotv']), writes=['moep'])
            kb.dma('sp', 'moep', lambda e: e.dma_start(out=pidx, in_=dr['c_pidx']), writes=['moep'])
            kb.barrier()
            LG = sbr("LG", [128, NT, 36], F32)
            kb.dma('sp', 'lgin', lambda e: e.dma_start(out=LG, in_=lg_d.rearrange("t p n -> p t n")), writes=['LG'])
            def t3(name, a, b_, dt=F32):
                return sb(name, [128, NT, a, b_] if b_ else [128, NT, a], dt)
            lg = LG[:, :, 0:4]
            le = LG[:, :, 4:36].rearrange("p t (g j) -> p t g j", j=8)
            gmax = sbr("gmax", [128, NT], F32)
            G1 = sbr("G1", [128, NT, 4], F32)
            dl = sbr("dl", [128, NT, 4], F32)
            sume = sbr("sume", [128, NT], F32)
            pg = sbr("pg", [128, NT], F32)
            prod = sbr("prod", [128, NT, 4, 8], F32)
            ein = sbr("ein", [128, NT, 8], F32)
            ein2 = sbr("ein2", [128, NT, 8], F32)
            m1 = sbr("m1", [128, NT], F32); m2 = sbr("m2", [128, NT], F32)
            oh = [sbr("oh%d" % k, [128, NT, 8], F32) for k in range(2)]
            OH = [sbr("OH%d" % k, [128, NT, 4, 8], F32) for k in range(2)]
            OHB = [sbr("OHB%d" % k, [128, NT, 32], BF16) for k in range(2)]

            def bc3(ap, n):
                return ap.unsqueeze(2).to_broadcast([128, NT, n])
            V = 'dve'
            kb.op(V, lambda e: e.reduce_max(out=gmax, in_=lg, axis=AX.X), ['LG'], ['gmax'])
            kb.op(V, lambda e: e.tensor_tensor(out=G1, in0=lg, in1=bc3(gmax, 4), op=ALU.is_equal), ['LG', 'gmax'], ['G1'])
            kb.op(V, lambda e: e.tensor_tensor(out=dl, in0=lg, in1=bc3(gmax, 4), op=ALU.subtract), ['LG', 'gmax'], ['dl'])
            kb.op('act', lambda e: e.activation(out=dl, in_=dl, func=AF.Exp), ['dl'], ['dl'])
            kb.op(V, lambda e: e.reduce_sum(out=sume, in_=dl, axis=AX.X), ['dl'], ['sume'])
            kb.op(V, lambda e: e.reciprocal(out=pg, in_=sume), ['sume'], ['pg'])
            kb.op(V, lambda e: e.tensor_tensor(out=prod, in0=le, in1=G1.unsqueeze(3).to_broadcast([128, NT, 4, 8]), op=ALU.mult), ['LG', 'G1'], ['prod'])
            kb.op(V, lambda e: e.reduce_sum(out=ein, in_=prod.rearrange("p t g j -> p t j g"), axis=AX.X), ['prod'], ['ein'])
            kb.op(V, lambda e: e.reduce_max(out=m1, in_=ein, axis=AX.X), ['ein'], ['m1'])
            kb.op(V, lambda e: e.tensor_tensor(out=oh[0], in0=ein, in1=bc3(m1, 8), op=ALU.is_equal), ['ein', 'm1'], ['oh0'])
            kb.op(V, lambda e: e.scalar_tensor_tensor(out=ein2, in0=oh[0], scalar=-1e30, in1=ein, op0=ALU.mult, op1=ALU.add), ['oh0', 'ein'], ['ein2'])
            kb.op(V, lambda e: e.reduce_max(out=m2, in_=ein2, axis=AX.X), ['ein2'], ['m2'])
            kb.op(V, lambda e: e.tensor_tensor(out=oh[1], in0=ein2, in1=bc3(m2, 8), op=ALU.is_equal), ['ein2', 'm2'], ['oh1'])
            kb.op(V, lambda e: e.tensor_tensor(out=m1, in0=m1, in1=m2, op=ALU.subtract), ['m1', 'm2'], ['m1'])
            kb.op('act', lambda e: e.activation(out=m1, in_=m1, func=AF.Sigmoid), ['m1'], ['m1'])
            kb.op(V, lambda e: e.tensor_tensor(out=GATE[0], in0=m1, in1=pg, op=ALU.mult), ['m1', 'pg'], ['gate0'])
            kb.op(V, lambda e: e.tensor_tensor(out=GATE[1], in0=pg, in1=GATE[0], op=ALU.subtract), ['pg', 'gate0'], ['gate1'])
            for k in range(2):
                kb.op(V, lambda e, k=k: e.tensor_tensor(out=OH[k], in0=G1.unsqueeze(3).to_broadcast([128, NT, 4, 8]),
                                                        in1=oh[k].unsqueeze(2).to_broadcast([128, NT, 4, 8]), op=ALU.mult),
                      ['G1', 'oh%d' % k], ['OH%d' % k])
                kb.op(V, lambda e, k=k: e.tensor_copy(out=OHB[k], in_=OH[k].rearrange("p t g j -> p t (g j)")), ['OH%d' % k], ['OHB%d' % k])

            TOT = sbr("TOT", [128, 2, NT, 32], F32)
            CA = sbr("CA", [128, 2, NT, 32], F32)
            CBf = sbr("CBf", [128, 2, NT, 32], F32)
            cnt = sbr("cnt", [128, 32], F32); pad = sbr("pad", [128, 32], F32)
            padi = sbr("padi", [128, 32], I32)
            pe_a = sbr("pe_a", [128, 32], F32); pe_b = sbr("pe_b", [128, 32], F32)
            base = sbr("base", [128, 2, 32], F32)
            OFF = sbr("OFF", [128, NT, 32], F32)
            DESTF = sbr("DESTF", [128, 2, NT], F32)
            for k in range(2):
                for hf in range(2):
                    mm(PS[4 + hf][:, 0:512], onesb, OHB[k].rearrange("p t e -> p (t e)")[:, hf * 512:(hf + 1) * 512], True, True,
                       ['OHB%d' % k, 'onesb'], [pkey(4 + hf)])
                    kb.op(V, lambda e, k=k, hf=hf: e.tensor_copy(out=TOT[:, k, hf * 16:(hf + 1) * 16, :].rearrange("p t e -> p (t e)"), in_=PS[4 + hf][:, 0:512]),
                          [pkey(4 + hf)], ['TOT'])
            kb.op(V, lambda e: e.tensor_copy(out=CA, in_=TOT), ['TOT'], ['CA'])
            src, dst, sk, dk = CA, CBf, 'CA', 'CB'
            sh = 1
            while sh < NT:
                kb.op(V, lambda e, src=src, dst=dst, sh=sh: e.tensor_copy(out=dst[:, :, 0:sh, :], in_=src[:, :, 0:sh, :]), [sk], [dk])
                kb.op(V, lambda e, src=src, dst=dst, sh=sh: e.tensor_tensor(out=dst[:, :, sh:NT, :], in0=src[:, :, sh:NT, :], in1=src[:, :, 0:NT - sh, :], op=ALU.add), [sk], [dk])
                src, dst, sk, dk = dst, src, dk, sk
                sh *= 2
            CUM, cumk = src, sk
            kb.op(V, lambda e: e.tensor_tensor(out=cnt, in0=CUM[:, 0, NT - 1, :], in1=CUM[:, 1, NT - 1, :], op=ALU.add), [cumk], ['cnt'])
            kb.op(V, lambda e: e.tensor_scalar(out=padi, in0=cnt, scalar1=float(RB - 1), scalar2=None, op0=ALU.add), ['cnt'], ['padi'])
            kb.op(V, lambda e: e.tensor_scalar(out=padi, in0=padi, scalar1=9, scalar2=9, op0=ALU.arith_shift_right, op1=ALU.logical_shift_left), ['padi'], ['padi'])
            kb.op(V, lambda e: e.tensor_copy(out=pad, in_=padi), ['padi'], ['pad'])
            kb.op(V, lambda e: e.tensor_copy(out=pe_a, in_=pad), ['pad'], ['pe_a'])
            src, dst, sk, dk = pe_a, pe_b, 'pe_a', 'pe_b'
            sh = 1
            while sh < 32:
                kb.op(V, lambda e, src=src, dst=dst, sh=sh: e.tensor_copy(out=dst[:, 0:sh], in_=src[:, 0:sh]), [sk], [dk])
                kb.op(V, lambda e, src=src, dst=dst, sh=sh: e.tensor_tensor(out=dst[:, sh:32], in0=src[:, sh:32], in1=src[:, 0:32 - sh], op=ALU.add), [sk], [dk])
                src, dst, sk, dk = dst, src, dk, sk
                sh *= 2
            PEND, pendk = src, sk
            kb.op(V, lambda e: e.tensor_tensor(out=base[:, 0, :], in0=PEND, in1=pad, op=ALU.subtract), [pendk, 'pad'], ['base'])
            kb.op(V, lambda e: e.tensor_tensor(out=base[:, 1, :], in0=base[:, 0, :], in1=CUM[:, 0, NT - 1, :], op=ALU.add), ['base', cumk], ['base'])
            for k in range(2):
                kb.op(V, lambda e, k=k: e.tensor_tensor(out=OFF, in0=CUM[:, k, :, :], in1=TOT[:, k, :, :], op=ALU.subtract), [cumk, 'TOT'], ['OFF'])
                kb.op(V, lambda e, k=k: e.tensor_tensor(out=OFF, in0=OFF, in1=base[:, k, :].unsqueeze(1).to_broadcast([128, NT, 32]), op=ALU.add), ['OFF', 'base'], ['OFF'])
                for hf in range(2):
                    mm(PS[4 + hf][:, 0:512], trirank, OHB[k].rearrange("p t e -> p (t e)")[:, hf * 512:(hf + 1) * 512], True, True,
                       ['OHB%d' % k, 'c_trirank'], [pkey(4 + hf)])
                    kb.op(V, lambda e, hf=hf: e.tensor_tensor(out=OFF[:, hf * 16:(hf + 1) * 16, :].rearrange("p t e -> p (t e)"), in0=OFF[:, hf * 16:(hf + 1) * 16, :].rearrange("p t e -> p (t e)"),
                                                              in1=PS[4 + hf][:, 0:512], op=ALU.add), ['OFF', pkey(4 + hf)], ['OFF'])
                kb.op(V, lambda e, k=k: e.tensor_tensor(out=OFF, in0=OFF, in1=OH[k].rearrange("p t g j -> p t (g j)"), op=ALU.mult), ['OFF', 'OH%d' % k], ['OFF'])
                kb.op(V, lambda e, k=k: e.reduce_sum(out=DESTF[:, k, :], in_=OFF, axis=AX.X), ['OFF'], ['DESTF'])
            kb.op(V, lambda e: e.tensor_copy(out=DEST, in_=DESTF), ['DESTF'], ['DEST'])
            CMP = sbr("CMP", [128, NSLOT, 32], F32)
            SE = sbr("SE", [128, NSLOT], F32)
            SEO = sbr("SEO", [128, NSLOT], F32)
            kb.op(V, lambda e: e.tensor_tensor(out=CMP, in0=PEND.unsqueeze(1).to_broadcast([128, NSLOT, 32]), in1=slotv.unsqueeze(2).to_broadcast([128, NSLOT, 32]), op=ALU.is_le),
                  [pendk, 'moep'], ['CMP'])
            kb.op(V, lambda e: e.reduce_sum(out=SE, in_=CMP, axis=AX.X), ['CMP'], ['SE'])
            kb.op(V, lambda e: e.tensor_scalar(out=SEO, in0=SE, scalar1=float(NE) - 0.5, scalar2=1.0e6, op0=ALU.is_ge, op1=ALU.mult), ['SE'], ['SEO'])
            kb.op(V, lambda e: e.tensor_scalar(out=SE, in0=SE, scalar1=float(NE - 1), scalar2=float(l * NE), op0=ALU.min, op1=ALU.add), ['SE'], ['SE'])
            kb.op(V, lambda e: e.scalar_tensor_tensor(out=SE, in0=SE, scalar=128.0, in1=pidx.to_broadcast([128, NSLOT]), op0=ALU.mult, op1=ALU.add), ['SE', 'moep'], ['SE'])
            kb.op(V, lambda e: e.tensor_tensor(out=SE, in0=SE, in1=SEO, op=ALU.add), ['SE', 'SEO'], ['SE'])
            kb.op(V, lambda e: e.tensor_copy(out=WIDX, in_=SE), ['SE'], ['WIDX'])

            kb.barrier()
            es_r.close()
            XB = [sb("XBs%d" % i, [128, D], BF16) for i in range(2)]
            for t in range(NT):
                i = t % 2
                kb.dma('sp', 'mx%d' % i, lambda e, i=i, t=t: e.dma_start(out=XT[i], in_=src_d[t * 128:(t + 1) * 128, :]),
                       reads=[('xb', t)], writes=[('MX', i)])
                copy_op('act' if i else 'dve', XB[i], XT[i], [('MX', i)], [('XBs', i)])
                for k in range(2):
                    kb.dma('pool', 'scat%d' % i, lambda e, i=i, k=k, t=t: e.indirect_dma_start(
                        out=xs_d, out_offset=bass.IndirectOffsetOnAxis(ap=DEST[:, k, t:t + 1], axis=0), in_=XB[i], in_offset=None),
                        reads=[('XBs', i), 'DEST'], writes=[('xs', t, k)])

            kb.barrier()
            WS = [sb("WS%d" % i, [128, 6144], F32) for i in range(2)]
            WB = [sb("WB%d" % i, [128, 6144], BF16) for i in range(2)]
            XS = [sb("XS%d" % i, [128, NRT, D], BF16) for i in range(2)]
            XST = [sb("XST%d" % i, [128, 8, RB], BF16) for i in range(2)]
            SG = sb("SG", [128, 2, RB], F32)
            HT = sb("HT", [128, 2, RB], BF16)
            YS = [sb("YS%d" % i, [128, NRT, D], BF16) for i in range(2)]
            pq = [0]

            def slot_w(sl):
                i = sl % 2
                kb.dma('pool', 'wexp%d' % i, lambda e: e.indirect_dma_start(
                    out=WS[i], out_offset=None, in_=dr['wexp'], in_offset=bass.IndirectOffsetOnAxis(ap=WIDX[:, sl:sl + 1], axis=0),
                    bounds_check=wexp_bound, oob_is_err=False),
                    reads=['WIDX'], writes=[('WS', i)])

            def slot_x(sl):
                i = sl % 2
                kb.dma('sp', 'xsl%d' % i, lambda e: e.dma_start(out=XS[i], in_=xs_d[sl * RB:(sl + 1) * RB, :].rearrange("(r p) d -> p r d", p=128)),
                       reads=[], writes=[('XS', i)])

            def slot_T(sl):
                i = sl % 2
                for r in range(NRT):
                    tb_ = 2 if r % 2 == 0 else 7
                    tpx = PS[tb_].bitcast(BF16)
                    for kc in range(8):
                        kb.op('pe', lambda e, r=r, kc=kc, tpx=tpx: e.transpose(out=tpx[:, kc * 128:(kc + 1) * 128], in_=XS[i][:, r, kc * 128:(kc + 1) * 128], identity=ident),
                              [('XS', i), 'c_ident'], [pkey(tb_)])
                    copy_op('dve' if r % 2 else 'act', XST[i][:, :, r * 128:(r + 1) * 128], tpx.rearrange("p (k c) -> p k c", c=128), [pkey(tb_)], [('XST', i, r)])

            def slot_cast(sl):
                i = sl % 2
                kb.op('dve', lambda e: e.tensor_copy(out=WB[i][:, 0:2048], in_=WS[i][:, 0:2048]), [('WS', i)], [('WB1', i)])
                kb.op('act', lambda e: e.activation(out=WB[i][:, 2048:4096], in_=WS[i][:, 2048:4096], func=AF.Copy), [('WS', i)], [('WB3', i)])
                kb.op('dve', lambda e: e.tensor_copy(out=WB[i][:, 4096:5120], in_=WS[i][:, 4096:5120]), [('WS', i)], [('WB2a', i)])
                kb.op('act', lambda e: e.activation(out=WB[i][:, 5120:6144], in_=WS[i][:, 5120:6144], func=AF.Copy), [('WS', i)], [('WB2b', i)])

            def slot_H(sl):
                i = sl % 2
                W1 = WB[i][:, 0:2048].rearrange("p (k c) -> p k c", c=256)
                W3 = WB[i][:, 2048:4096].rearrange("p (k c) -> p k c", c=256)
                xk = [('XST', i, r_) for r_ in range(NRT)]
                for dc in range(2):
                    b1_, b3_ = (0, 1) if dc == 0 else (5, 6)
                    for kc in range(8):
                        mm(PS[b1_][:, 0:RB], W1[:, kc, dc * 128:(dc + 1) * 128], XST[i][:, kc, :], kc == 0, kc == 7, [('WB1', i)] + xk, [pkey(b1_)])
                    for kc in range(8):
                        mm(PS[b3_][:, 0:RB], W3[:, kc, dc * 128:(dc + 1) * 128], XST[i][:, kc, :], kc == 0, kc == 7, [('WB3', i)] + xk, [pkey(b3_)])
                for dc in range(2):
                    b1_, b3_ = (0, 1) if dc == 0 else (5, 6)
                    kb.op('act', lambda e, dc=dc, b1_=b1_: e.activation(out=SG[:, dc, :], in_=PS[b1_][:, 0:RB], func=AF.Silu), [pkey(b1_)], [('SG', dc)])
                    kb.op('dve', lambda e, dc=dc, b3_=b3_: e.tensor_tensor(out=HT[:, dc, :], in0=SG[:, dc, :], in1=PS[b3_][:, 0:RB], op=ALU.mult), [('SG', dc), pkey(b3_)], [('HT', dc)])

            def slot_Y(sl):
                i = sl % 2
                W2 = WB[i][:, 4096:6144].rearrange("p (j c) -> p j c", c=1024)
                for r in range(NRT):
                    for half in range(2):
                        pq[0] += 1
                        b = 3 + pq[0] % 2
                        for dc in range(2):
                            mm(PS[b][:, 0:512], HT[:, dc, r * 128:(r + 1) * 128], W2[:, dc, half * 512:(half + 1) * 512], dc == 0, dc == 1,
                               [('HT', dc), ('WB2a', i), ('WB2b', i)], [pkey(b)])
                        copy_op('act' if half else 'dve', YS[i][:, r, half * 512:(half + 1) * 512], PS[b][:, 0:512], [pkey(b)], [('YS', i)])
                kb.dma('sp', 'yso%d' % i, lambda e: e.dma_start(out=ys_d[sl * RB:(sl + 1) * RB, :].rearrange("(r p) d -> p r d", p=128), in_=YS[i]),
                       reads=[('YS', i)], writes=[('ys', sl)])

            slot_w(0)
            slot_x(0)
            slot_x(1)
            slot_T(0)
            for sl in range(NSLOT):
                if sl + 1 < NSLOT:
                    slot_w(sl + 1)
                if sl + 2 < NSLOT:
                    slot_x(sl + 2)
                slot_cast(sl)
                slot_H(sl)
                if sl + 1 < NSLOT:
                    slot_T(sl + 1)
                slot_Y(sl)
            kb.barrier()
            G0 = [sb("G0_%d" % i, [128, D], BF16) for i in range(2)]
            A0 = [sb("A0_%d" % i, [128, D], F32) for i in range(2)]
            YO = [sb("YO_%d" % i, [128, D], F32) for i in range(2)]
            G1g = [sb("G1_%d" % i, [128, D], BF16) for i in range(2)]
            lns = [(sb("mst%d" % i, [128, 12], F32), sb("mmv%d" % i, [128, 2], F32), sb("mrs%d" % i, [128, 1], F32),
                    sb("mnm%d" % i, [128, 1], F32)) for i in range(3)]
            def comb_loads(t):
                i, x3 = t % 2, t % 3
                kb.dma('sp', 'cmx%d' % x3, lambda e: e.dma_start(out=XT[x3], in_=src_d[t * 128:(t + 1) * 128, :]),
                       reads=[('xb', t)], writes=[('MX', x3)])
                kb.dma('pool', 'cg0_%d' % i, lambda e: e.indirect_dma_start(out=G0[i], out_offset=None, in_=ys_d,
                       in_offset=bass.IndirectOffsetOnAxis(ap=DEST[:, 0, t:t + 1], axis=0)), reads=['DEST'], writes=[('G0', i)])
                kb.dma('pool', 'cg1_%d' % i, lambda e: e.indirect_dma_start(out=G1g[i], out_offset=None, in_=ys_d,
                       in_offset=bass.IndirectOffsetOnAxis(ap=DEST[:, 1, t:t + 1], axis=0)), reads=['DEST'], writes=[('G1g', i)])

            XBc1 = [sb("cbxb%d" % i, [128, D], BF16) for i in range(2)]
            XTs1 = [sb("cbxs%d" % i, [128, 8, 128], BF16) for i in range(2)]

            def cb_fin(tt):
                ii, x3 = tt % 2, tt % 3
                ln_apply(lns[x3], XT[x3], ('MX', x3), YO[ii], ('YO', ii), g2, b2, 'moep', 'ln2_%d' % x3, gain_engine='dve' if tt % 2 else 'pool')
                kb.dma('sp', 'mo%d' % ii, lambda e: e.dma_start(out=dst_d[tt * 128:(tt + 1) * 128, :], in_=YO[ii]),
                       reads=[('YO', ii)], writes=[('xa', tt)])
            comb_loads(0)
            for t in range(NT):
                i, x3 = t % 2, t % 3
                if t + 1 < NT:
                    comb_loads(t + 1)
                kb.op('act', lambda e, i=i, t=t: e.activation(out=A0[i], in_=G0[i], func=AF.Identity, scale=GATE[0][:, t:t + 1]),
                      [('G0', i), 'gate0'], [('A0', i)])
                kb.op('dve', lambda e, i=i, x3=x3: e.scalar_tensor_tensor(out=XT[x3], in0=XT[x3], scalar=ALPHA, in1=A0[i], op0=ALU.mult, op1=ALU.add),
                      [('MX', x3), ('A0', i)], [('MX', x3)])
                kb.op('dve', lambda e, i=i, t=t, x3=x3: e.scalar_tensor_tensor(out=XT[x3], in0=G1g[i], scalar=GATE[1][:, t:t + 1], in1=XT[x3], op0=ALU.mult, op1=ALU.add),
                      [('MX', x3), ('G1g', i), 'gate1'], [('MX', x3)])
                ln_stats(lns[x3], XT[x3], ('MX', x3), 'ln2_%d' % x3)
                if t >= 1:
                    cb_fin(t - 1)
                if t >= 2 and l + 1 < nlayers:
                    emit_xt_tile(YO[t % 2], ('YO', t % 2), t - 2, XBc1, XTs1, t % 2)
            cb_fin(NT - 1)
            if l + 1 < nlayers:
                emit_xt_tile(YO[NT % 2], ('YO', NT % 2), NT - 2, XBc1, XTs1, 0)
                emit_xt_tile(YO[(NT - 1) % 2], ('YO', (NT - 1) % 2), NT - 1, XBc1, XTs1, 1)
        kb.barrier()

    def copy_stream(src_d, dst_d):
        with ExitStack() as es:
            sb = mk_sb(es)
            Cb = [sb("cpy%d" % i, [128, 4, D], F32) for i in range(2)]
            for t in range(NT // 4):
                i = t % 2
                kb.dma('sp', 'cpi%d' % i, lambda e, i=i, t=t: e.dma_start(out=Cb[i], in_=src_d[t * 512:(t + 1) * 512, :].rearrange("(r p) d -> p r d", p=128)), writes=[('cpy', i)])
                kb.dma('sp', 'cpo%d' % i, lambda e, i=i, t=t: e.dma_start(out=dst_d[t * 512:(t + 1) * 512, :].rearrange("(r p) d -> p r d", p=128), in_=Cb[i]), reads=[('cpy', i)])
        kb.barrier()

    if phases is None:
        phases = ['ln0'] + sum([['mix%d' % l, 'moe%d' % l] for l in range(nlayers)], [])
    kb.barrier()
    cur = None
    for ph in phases:
        if ph == 'ln0':
            phase_ln0(xa_d); cur = xa_d
        elif ph == 'in_xa':
            copy_stream(dr['x'], xa_d); cur = xa_d
        elif ph == 'in_xb':
            copy_stream(dr['x'], xb_d); cur = xb_d
        elif ph.startswith('mix'):
            phase_mix(int(ph[3:]), xa_d, xb_d); cur = xb_d
        elif ph.startswith('moe'):
            last = (ph == phases[-1])
            phase_moe(int(ph[3:]), xb_d, y_d if last else xa_d); cur = y_d if last else xa_d
    if cur is not y_d:
        copy_stream(cur, y_d)
    kb.finish()
    es_global.close()
    return nc, kb


def _prep_shared(inp):
    f = lambda a: np.ascontiguousarray(np.asarray(a, dtype=np.float32))
    rep = lambda v: f(np.broadcast_to(np.asarray(v)[None, :], (128, np.asarray(v).shape[-1])))
    repl = lambda v: f(np.broadcast_to(np.asarray(v)[:, None, :], (L, 128, np.asarray(v).shape[-1])))
    sh = {}
    sh['ln_in_g'] = rep(inp['ln_in_g']); sh['ln_in_b'] = rep(inp['ln_in_b'])
    sh['w_in'] = f(inp['w_in'])
    sh['nb_f'] = f(np.asarray(inp['b_f']).reshape(L, 4, 1))
    sh['conv_w'] = f(np.asarray(inp['conv_w']).reshape(L, 3, 2, 128).transpose(0, 3, 2, 1))
    sh['conv_b'] = f(np.asarray(inp['conv_b']).reshape(L, 2, 128).transpose(0, 2, 1))
    sh['sgu_ln_g'] = repl(inp['sgu_ln_g']); sh['sgu_ln_b'] = repl(inp['sgu_ln_b'])
    sh['sgu_w'] = f(inp['sgu_w'])
    sh['sgu_b'] = f(np.asarray(inp['sgu_b']).reshape(L, 2, 2, 128))
    sh['grp_g'] = f(np.asarray(inp['grp_g']).reshape(L, 8, 128).transpose(0, 2, 1))
    sh['w_out'] = f(inp['w_out'])
    sh['ln1_g'] = repl(inp['ln1_g']); sh['ln1_b'] = repl(inp['ln1_b'])
    sh['router_w'] = f(np.concatenate([np.asarray(inp['router_g_w']), np.asarray(inp['router_e_w'])], axis=-1))
    sh['router_b'] = repl(np.concatenate([np.asarray(inp['router_g_b']), np.asarray(inp['router_e_b'])], axis=-1))
    w1 = np.asarray(inp['w1']).reshape(L, NE, 8, 128, 256).transpose(0, 1, 3, 2, 4).reshape(L, NE, 128, 2048)
    w3 = np.asarray(inp['w3']).reshape(L, NE, 8, 128, 256).transpose(0, 1, 3, 2, 4).reshape(L, NE, 128, 2048)
    w2 = np.asarray(inp['w2']).reshape(L, NE, 2, 128, 1024).transpose(0, 1, 3, 2, 4).reshape(L, NE, 128, 2048)
    sh['wexp'] = f(np.concatenate([w1, w3, w2], axis=-1).reshape(L * NE * 128, 6144))
    sh['ln2_g'] = repl(inp['ln2_g']); sh['ln2_b'] = repl(inp['ln2_b'])
    sh.update(_consts())
    return sh


_CACHE = {}


def kernel(**inputs):
    x = np.asarray(inputs['x'], dtype=np.float32)
    sh = _prep_shared(inputs)
    if 'nc' not in _CACHE:
        _CACHE['nc'] = build_program()[0]
    nc = _CACHE['nc']
    in_maps = []
    for c in range(NCORES):
        m = dict(sh)
        m['x'] = np.ascontiguousarray(x[c * NSEQ:(c + 1) * NSEQ].reshape(T, D))
        in_maps.append(m)
    res = run_bass_kernel_spmd(nc, in_maps, core_ids=list(range(NCORES)))
    out = np.stack([np.asarray(r['y']).reshape(NSEQ, S, D) for r in res.results], axis=0)
    return out.reshape(NCORES * NSEQ, S, D).astype(np.float32)
```
